# Optimizing a Trainium2 kernel written in Bass

```python
import math
import jax, jax.numpy as jnp
from jax import lax
import numpy as np

D_MODEL = 4096
BATCH = 2
SEQ = 8192
DEPTH = 2

D_MIX = D_MODEL
ATT_WIDTH = D_MIX // 2
ATT_HEAD_DIM = 128
ATT_HEADS = ATT_WIDTH // (2 * ATT_HEAD_DIM)
Q_BLOCK = 128
SSM_WIDTH = D_MIX - ATT_WIDTH
SSM_HEAD_DIM = 64
SSM_HEADS = SSM_WIDTH // SSM_HEAD_DIM
SSM_GROUPS = 8
SSM_HEADS_PER_GROUP = SSM_HEADS // SSM_GROUPS
SSM_STATE = 128
CONV_K = 4
CONV_CH = SSM_WIDTH + 2 * SSM_GROUPS * SSM_STATE
SSM_CHUNK = 128
IN_COLS = 3 * ATT_WIDTH + SSM_WIDTH + CONV_CH + SSM_HEADS
D_FF = 11008
N_EXPERTS = 8
TOP_K = 2
MOE_D_FF = 5632
N_DENSE = (DEPTH + 1) // 2
N_MOE = DEPTH // 2
EPS = 1e-6

kernel_name = "hybrid_diffattn_ssd_moe_trunk"


def rms_norm(x, w):
    xf = x.astype(jnp.float32)
    out = xf * lax.rsqrt(jnp.mean(xf * xf, axis=-1, keepdims=True) + EPS)
    return (out * w.astype(jnp.float32)).astype(x.dtype)


def swiglu(x, w_gate, w_up, w_down):
    return (jax.nn.silu(x @ w_gate) * (x @ w_up)) @ w_down


def lambda_init_for(layer):
    return 0.8 - 0.6 * math.exp(-0.3 * layer)


def diff_attention(q, k, v, q_norm_w, k_norm_w, lam, lambda_init, subln_w):
    b, s = q.shape[:2]
    q = rms_norm(q, q_norm_w)
    k = rms_norm(k, k_norm_w)
    scale = ATT_HEAD_DIM ** -0.5
    n_blk = s // Q_BLOCK
    q_blocks = jnp.moveaxis(q.reshape(b, n_blk, Q_BLOCK, ATT_HEADS, 2, ATT_HEAD_DIM), 1, 0)
    k_pos = jnp.arange(s)

    def attend_block(args):
        qb, blk = args
        scores = jnp.einsum('bqhcd,bkhcd->bhcqk', qb, k).astype(jnp.float32) * scale
        q_pos = blk * Q_BLOCK + jnp.arange(Q_BLOCK)
        causal = k_pos[None, :] <= q_pos[:, None]
        probs = jax.nn.softmax(jnp.where(causal, scores, -jnp.inf), axis=-1)
        weights = probs[:, :, 0] - lam * probs[:, :, 1]
        return jnp.einsum('bhqk,bkhe->bqhe', weights.astype(v.dtype), v)

    out = lax.map(attend_block, (q_blocks, jnp.arange(n_blk)))
    out = jnp.moveaxis(out, 0, 1).reshape(b, s, ATT_HEADS, 2 * ATT_HEAD_DIM)
    out = rms_norm(out, subln_w) * (1.0 - lambda_init)
    return out.reshape(b, s, ATT_WIDTH)


def causal_depthwise_conv(u, w, bias):
    s = u.shape[1]
    up = jnp.pad(u, ((0, 0), (CONV_K - 1, 0), (0, 0)))
    out = bias
    for tap in range(CONV_K):
        out = out + up[:, tap:tap + s] * w[tap]
    return out


def ssd_chunked_scan(xs, dt, a, bm, cm):
    b, s = xs.shape[:2]
    nc = s // SSM_CHUNK

    def chunks(t):
        return jnp.moveaxis(t.reshape((b, nc, SSM_CHUNK) + t.shape[2:]), 1, 0)

    causal = jnp.tril(jnp.ones((SSM_CHUNK, SSM_CHUNK), dtype=bool))[None, :, :, None, None]

    def step(state, inp):
        xc, dtc, bc, cc = inp
        acum = jnp.cumsum(dtc * a, axis=1)
        seg = acum[:, :, None] - acum[:, None, :]
        decay = jnp.exp(jnp.where(causal, seg, -jnp.inf))
        cb = jnp.einsum('blgn,bsgn->blsg', cc, bc)
        w = cb[..., None] * decay * dtc[:, None]
        y = jnp.einsum('blsgh,bsghp->blghp', w, xc)
        y = y + jnp.einsum('blgn,bghpn->blghp', cc, state) * jnp.exp(acum)[..., None]
        to_end = jnp.exp(acum[:, -1:] - acum) * dtc
        state = (state * jnp.exp(acum[:, -1])[..., None, None]
                 + jnp.einsum('blgn,blgh,blghp->bghpn', bc, to_end, xc))
        return state, y

    init = jnp.zeros((b, SSM_GROUPS, SSM_HEADS_PER_GROUP, SSM_HEAD_DIM, SSM_STATE), jnp.float32)
    _, y = lax.scan(step, init, (chunks(xs), chunks(dt), chunks(bm), chunks(cm)))
    return jnp.moveaxis(y, 0, 1).reshape(xs.shape)


def ssd_mixer(z, xbc, dt, conv_w, conv_b, dt_bias, a_log, d_skip, norm_w):
    b, s, _ = xbc.shape
    f32 = jnp.float32
    xbc = jax.nn.silu(causal_depthwise_conv(xbc, conv_w, conv_b))
    xs, bm, cm = jnp.split(xbc, [SSM_WIDTH, SSM_WIDTH + SSM_GROUPS * SSM_STATE], axis=-1)
    xs = xs.reshape(b, s, SSM_GROUPS, SSM_HEADS_PER_GROUP, SSM_HEAD_DIM).astype(f32)
    bm = bm.reshape(b, s, SSM_GROUPS, SSM_STATE).astype(f32)
    cm = cm.reshape(b, s, SSM_GROUPS, SSM_STATE).astype(f32)
    dt = jax.nn.softplus(dt.astype(f32) + dt_bias.astype(f32))
    dt = dt.reshape(b, s, SSM_GROUPS, SSM_HEADS_PER_GROUP)
    a = -jnp.exp(a_log.astype(f32)).reshape(SSM_GROUPS, SSM_HEADS_PER_GROUP)
    y = ssd_chunked_scan(xs, dt, a, bm, cm)
    y = y + d_skip.astype(f32).reshape(SSM_GROUPS, SSM_HEADS_PER_GROUP, 1) * xs
    y = y.reshape(b, s, SSM_WIDTH).astype(z.dtype)
    gated = (y * jax.nn.silu(z)).reshape(b, s, SSM_GROUPS, SSM_WIDTH // SSM_GROUPS)
    gated = rms_norm(gated, norm_w.reshape(SSM_GROUPS, SSM_WIDTH // SSM_GROUPS))
    return gated.reshape(b, s, SSM_WIDTH)


def moe_swiglu(h, router_w, w_gate, w_up, w_down):
    b, s, d = h.shape
    xt = h.reshape(b * s, d)
    logits = (xt @ router_w).astype(jnp.float32)
    top_vals, top_idx = lax.top_k(logits, TOP_K)
    top_gates = jax.nn.softmax(top_vals, axis=-1)
    combine = jnp.sum(jax.nn.one_hot(top_idx, N_EXPERTS, dtype=jnp.float32)
                      * top_gates[..., None], axis=1)
    out = jnp.zeros_like(xt)
    for e in range(N_EXPERTS):
        out = out + combine[:, e:e + 1].astype(xt.dtype) * swiglu(xt, w_gate[e], w_up[e], w_down[e])
    return out.reshape(b, s, d)


def setup_inputs(seed: int = 0) -> dict:
    key = jax.random.key(seed)
    ks = jax.random.split(key, 28)
    f32 = jnp.float32

    def normal(k, shape, scale):
        return jax.random.normal(k, shape, f32) * scale

    def gain(k, shape):
        return 1.0 + 0.02 * jax.random.normal(k, shape, f32)

    dt0 = jnp.exp(jax.random.uniform(ks[14], (DEPTH, SSM_HEADS), f32,
                                     minval=math.log(1e-3), maxval=math.log(1e-1)))
    return {
        "x": normal(ks[0], (BATCH, SEQ, D_MODEL), 1.0),
        "norm1_w": gain(ks[1], (DEPTH, D_MODEL)),
        "w_in": normal(ks[2], (DEPTH, D_MODEL, IN_COLS), D_MODEL ** -0.5),
        "w_out": normal(ks[3], (DEPTH, D_MIX, D_MODEL), D_MIX ** -0.5),
        "q_norm_w": gain(ks[4], (DEPTH, ATT_HEAD_DIM)),
        "k_norm_w": gain(ks[5], (DEPTH, ATT_HEAD_DIM)),
        "lambda_q1": normal(ks[6], (DEPTH, ATT_HEAD_DIM), 0.1),
        "lambda_k1": normal(ks[7], (DEPTH, ATT_HEAD_DIM), 0.1),
        "lambda_q2": normal(ks[8], (DEPTH, ATT_HEAD_DIM), 0.1),
        "lambda_k2": normal(ks[9], (DEPTH, ATT_HEAD_DIM), 0.1),
        "subln_w": gain(ks[10], (DEPTH, 2 * ATT_HEAD_DIM)),
        "conv_w": normal(ks[11], (DEPTH, CONV_K, CONV_CH), CONV_K ** -0.5),
        "conv_b": normal(ks[12], (DEPTH, CONV_CH), 0.02),
        "dt_bias": dt0 + jnp.log(-jnp.expm1(-dt0)),
        "a_log": jnp.log(jax.random.uniform(ks[15], (DEPTH, SSM_HEADS), f32, minval=1.0, maxval=16.0)),
        "d_skip": gain(ks[16], (DEPTH, SSM_HEADS)),
        "ssm_norm_w": gain(ks[17], (DEPTH, SSM_WIDTH)),
        "norm2_w": gain(ks[18], (DEPTH, D_MODEL)),
        "ffn_w_gate": normal(ks[19], (N_DENSE, D_MODEL, D_FF), D_MODEL ** -0.5),
        "ffn_w_up": normal(ks[20], (N_DENSE, D_MODEL, D_FF), D_MODEL ** -0.5),
        "ffn_w_down": normal(ks[21], (N_DENSE, D_FF, D_MODEL), D_FF ** -0.5),
        "router_w": normal(ks[22], (N_MOE, D_MODEL, N_EXPERTS), D_MODEL ** -0.5),
        "moe_w_gate": normal(ks[23], (N_MOE, N_EXPERTS, D_MODEL, MOE_D_FF), D_MODEL ** -0.5),
        "moe_w_up": normal(ks[24], (N_MOE, N_EXPERTS, D_MODEL, MOE_D_FF), D_MODEL ** -0.5),
        "moe_w_down": normal(ks[25], (N_MOE, N_EXPERTS, MOE_D_FF, D_MODEL), MOE_D_FF ** -0.5),
    }


def reference(x, norm1_w, w_in, w_out, q_norm_w, k_norm_w, lambda_q1, lambda_k1, lambda_q2,
              lambda_k2, subln_w, conv_w, conv_b, dt_bias, a_log, d_skip, ssm_norm_w, norm2_w,
              ffn_w_gate, ffn_w_up, ffn_w_down, router_w, moe_w_gate, moe_w_up, moe_w_down):
    b, s, _ = x.shape
    splits = [ATT_WIDTH, 2 * ATT_WIDTH, 3 * ATT_WIDTH, 3 * ATT_WIDTH + SSM_WIDTH,
              3 * ATT_WIDTH + SSM_WIDTH + CONV_CH]
    for layer in range(DEPTH):
        h = rms_norm(x, norm1_w[layer])
        proj = h @ w_in[layer]
        q, k, v, z, xbc, dt = jnp.split(proj, splits, axis=-1)
        lam_init = lambda_init_for(layer)
        lam = (jnp.exp(jnp.sum(lambda_q1[layer] * lambda_k1[layer]).astype(jnp.float32))
               - jnp.exp(jnp.sum(lambda_q2[layer] * lambda_k2[layer]).astype(jnp.float32))
               + lam_init)
        att = diff_attention(q.reshape(b, s, ATT_HEADS, 2, ATT_HEAD_DIM),
                             k.reshape(b, s, ATT_HEADS, 2, ATT_HEAD_DIM),
                             v.reshape(b, s, ATT_HEADS, 2 * ATT_HEAD_DIM),
                             q_norm_w[layer], k_norm_w[layer], lam, lam_init, subln_w[layer])
        ssm = ssd_mixer(z, xbc, dt, conv_w[layer], conv_b[layer], dt_bias[layer], a_log[layer],
                        d_skip[layer], ssm_norm_w[layer])
        x = x + jnp.concatenate([att, ssm], axis=-1) @ w_out[layer]
        h = rms_norm(x, norm2_w[layer])
        if layer % 2 == 0:
            i = layer // 2
            x = x + swiglu(h, ffn_w_gate[i], ffn_w_up[i], ffn_w_down[i])
        else:
            i = layer // 2
            x = x + moe_swiglu(h, router_w[i], moe_w_gate[i], moe_w_up[i], moe_w_down[i])
    return x
```

```python
import numpy as np
from contextlib import ExitStack
import concourse.bass as bass
import concourse.mybir as mybir

F32 = mybir.dt.float32
BF16 = mybir.dt.bfloat16
I32 = mybir.dt.int32
AF = mybir.ActivationFunctionType
ALU = mybir.AluOpType
AX = mybir.AxisListType

ENGS = ("pe", "act", "dve", "pool", "sp")


class Buf:
    def __init__(self, name):
        self.name = name
        self.w = None
        self.r = []
        self.dsem = None


class Rec:
    def __getattr__(self, name):
        def f(*a, **k):
            self.call = (name, a, k)
        return f


def _rec(fn):
    r = Rec(); fn(r)
    return r.call


class Prog:
    def __init__(self, nc):
        self.nc = nc
        self.es = ExitStack()
        self.pes = None
        self.ops = {e: [] for e in ENGS}
        self.cnt = {e: 0 for e in ENGS}
        self.sem = {e: self.es.enter_context(nc.semaphore("s_" + e)) for e in ENGS}
        self.dcnt = {}
        self.known = {e: {} for e in ENGS}
        self.bufs = []
        self.nd = 0

    def sb(self, name, shape, dt):
        es = self.pes if self.pes is not None else self.es
        self.nd += 1; name = "%s_u%d" % (name, self.nd)
        t = es.enter_context(self.nc.sbuf_tensor(name, list(shape), dt))
        b = Buf(name); b.t = t; self.bufs.append(b)
        return b

    def ps(self, name, shape, dt=F32):
        t = self.es.enter_context(self.nc.psum_tensor(name, list(shape), dt))
        b = Buf(name); b.t = t; self.bufs.append(b)
        return b

    def dram(self, name, shape, dt, kind="Internal", **kw):
        t = self.nc.dram_tensor(name, list(shape), dt, kind=kind, **kw)
        b = Buf(name); b.t = t; self.bufs.append(b)
        return b

    def _dsem(self, b):
        if b.dsem is None:
            b.dsem = self.es.enter_context(self.nc.semaphore("d%d" % self.nd)); self.nd += 1
            self.dcnt[b.dsem] = 0
        return b.dsem

    def _deps(self, eng, reads, writes):
        deps = []
        for b in reads:
            if b.w is not None: deps.append(b.w)
        for b in writes:
            if b.w is not None: deps.append(b.w)
            deps.extend(b.r)
        waits = {}
        for (sem, val, dma) in deps:
            if dma:
                val = max(val, self.dcnt[sem])
            if sem is self.sem["pe"] and eng == "pe":
                continue
            if self.known[eng].get(sem, 0) >= val:
                continue
            waits[sem] = max(waits.get(sem, 0), val)
        for sem, val in waits.items():
            self.known[eng][sem] = val
        return list(waits.items())

    def op(self, eng, fn, reads=(), writes=()):
        waits = self._deps(eng, reads, writes)
        self.cnt[eng] += 1
        tok = (self.sem[eng], self.cnt[eng], False)
        self.ops[eng].append((waits, _rec(fn), (self.sem[eng], 1)))
        for b in reads: b.r.append(tok)
        for b in writes: b.w = tok; b.r = []
        return tok

    def dma(self, q, fn, src, dst):
        waits = self._deps(q, [src], [dst])
        sem = self._dsem(dst)
        self.dcnt[sem] += 16
        tok = (sem, self.dcnt[sem], True)
        self.ops[q].append((waits, _rec(fn), (sem, 16)))
        src.r.append(tok)
        dst.w = tok; dst.r = []
        return tok

    def wait_all(self, eng, bufs):
        deps_w = self._deps(eng, bufs, bufs)
        if deps_w:
            self.ops[eng].append((deps_w, None, None))

    def barrier(self):
        for e in ENGS:
            waits = {}
            for e2 in ENGS:
                if self.cnt[e2] and self.known[e].get(self.sem[e2], 0) < self.cnt[e2] and e2 != e:
                    waits[self.sem[e2]] = self.cnt[e2]
            for sem, c in self.dcnt.items():
                if c and self.known[e].get(sem, 0) < c:
                    waits[sem] = c
            for sem, val in waits.items():
                self.known[e][sem] = val
            if waits:
                self.ops[e].append((list(waits.items()), None, None))

    def emit(self):
        nc = self.nc
        ops = self.ops
        with nc.Block() as block:
            def run(eng, lst):
                for waits, fn, inc in lst:
                    for sem, val in waits:
                        eng.wait_ge(sem, val)
                    if fn is not None:
                        ins = getattr(eng, fn[0])(*fn[1], **fn[2])
                        ins.then_inc(inc[0], inc[1])

            @block.tensor
            def _(e): run(e, ops["pe"])

            @block.scalar
            def _(e): run(e, ops["act"])

            @block.vector
            def _(e): run(e, ops["dve"])

            @block.gpsimd
            def _(e): run(e, ops["pool"])

            @block.sync
            def _(e): run(e, ops["sp"])
        self.ops = {e: [] for e in ENGS}

    def phase(self):
        prog = self
        class _Ph:
            def __enter__(s2):
                prog.pes = ExitStack()
            def __exit__(s2, *a):
                if a[0] is None:
                    prog.barrier()
                    prog.emit()
                prog.pes.close(); prog.pes = None
        return _Ph()

    def phase_begin(self):
        self.pes = ExitStack()

    def phase_end(self):
        self.barrier()
        self.emit()
        self.pes.close(); self.pes = None

    def finish(self):
        self.emit()
        self.es.close()


import numpy as np

HD = 128
VD = 256
NH = 2
NG = 2
HPG = 4
PD = 64
NS = 128
CK = 4
EPS = 1e-6


def consts_np():
    k = np.arange(128)
    mincl = (k[:, None] <= k[None, :]).astype(np.float32)
    ugt = (k[:, None] > k[None, :]).astype(np.float32)
    return {
        "c_ident": np.eye(128, dtype=np.float32),
        "c_ones": np.ones((128, 128), np.float32),
        "c_mincl": mincl,
        "c_ugt": ugt,
    }


def build_mixer(nc, D, S, lam_init):
    import ml_dtypes
    p = Prog(nc)
    DC = D // 128
    TT = 512 if S >= 512 else S
    NT = S // TT
    NB = S // 128
    din = lambda name, shape, dt=F32: p.dram(name, shape, dt, kind="ExternalInput")
    hT = din("hT", [D, S], BF16)
    wqk = din("wqk", [D, 2 * NH * 2 * HD])
    wxbc = din("wxbc", [D, NG * HPG * PD + 2 * NG * NS])
    wtok = din("wtok", [D, NH * VD + NG * HPG * PD + NG * HPG])
    NXB = NG * HPG * PD + 2 * NG * NS
    NXC = NXB // 128
    cw = din("conv_w", [128, NXC, CK])
    cb = din("conv_b", [128, NXC])
    qkw = din("qkw", [128, 2])
    lamv = din("lamv", [128, 4 * HD])
    sublnw = din("sublnw", [128, VD])
    dtb = din("dtb", [128, NG * HPG])
    alog = din("alog", [128, NG * HPG])
    dsk = din("dsk", [128, NG * HPG])
    snw = din("snw", [128, NG * HPG * PD])
    cid = din("c_ident", [128, 128]); cones = din("c_ones", [128, 128])
    cmin = din("c_mincl", [128, 128]); cug = din("c_ugt", [128, 128])
    mix = p.dram("mix", [S, NH * VD + NG * HPG * PD], BF16, kind="ExternalOutput")
    QT = p.dram("QT", [2 * NH, 128, S], BF16); KT = p.dram("KT", [2 * NH, 128, S], BF16)
    XBC = p.dram("XBC", [NXC, 128, S], BF16)
    VE = p.dram("VE", [S, NH, VD + 1], BF16)
    SZ = p.dram("SZ", [S, NG * HPG * PD], BF16)
    DT = p.dram("DTs", [S, NG * HPG], F32)

    def ld(name, src, shape, dt=F32, q="sp"):
        b = p.sb(name, shape, dt)
        p.dma(q, lambda e: e.dma_start(out=b.t[:], in_=src.t[:]), src, b)
        return b
    ones_f = ld("ones_f", cones, [128, 128]); mincl_f = ld("mincl_f", cmin, [128, 128]); ugt_f = ld("ugt_f", cug, [128, 128])
    ident_f = ld("ident_f", cid, [128, 128])
    ident_b = p.sb("ident_b", [128, 128], BF16); mincl_b = p.sb("mincl_b", [128, 128], BF16); ones_b = p.sb("ones_b", [128, 128], BF16)
    p.op("dve", lambda e: e.tensor_copy(out=ident_b.t[:], in_=ident_f.t[:]), [ident_f], [ident_b])
    p.op("dve", lambda e: e.tensor_copy(out=mincl_b.t[:], in_=mincl_f.t[:]), [mincl_f], [mincl_b])
    p.op("dve", lambda e: e.tensor_copy(out=ones_b.t[:], in_=ones_f.t[:]), [ones_f], [ones_b])
    cw_s = ld("cw_s", cw, [128, NXC, CK]); cb_s = ld("cb_s", cb, [128, NXC]); qkw_s = ld("qkw_s", qkw, [128, 2])
    lam_s = ld("lam_s", lamv, [128, 4 * HD]); sub_s = ld("sub_s", sublnw, [128, VD])
    dtb_s = ld("dtb_s", dtb, [128, NG * HPG]); alog_s = ld("alog_s", alog, [128, NG * HPG]); dsk_s = ld("dsk_s", dsk, [128, NG * HPG])
    snw_s = ld("snw_s", snw, [128, NG * HPG * PD])
    ltmp = p.sb("ltmp", [128, 2 * HD], F32); lsum = p.sb("lsum", [128, 2], F32); neglam = p.sb("neglam", [128, 1], F32)
    p.op("dve", lambda e: e.tensor_tensor(out=ltmp.t[:, 0:HD], in0=lam_s.t[:, 0:HD], in1=lam_s.t[:, HD:2 * HD], op=ALU.mult), [lam_s], [ltmp])
    p.op("dve", lambda e: e.tensor_tensor(out=ltmp.t[:, HD:2 * HD], in0=lam_s.t[:, 2 * HD:3 * HD], in1=lam_s.t[:, 3 * HD:4 * HD], op=ALU.mult), [lam_s], [ltmp])
    p.op("dve", lambda e: e.tensor_reduce(out=lsum.t[:, 0:1], in_=ltmp.t[:, 0:HD], axis=AX.X, op=ALU.add), [ltmp], [lsum])
    p.op("dve", lambda e: e.tensor_reduce(out=lsum.t[:, 1:2], in_=ltmp.t[:, HD:2 * HD], axis=AX.X, op=ALU.add), [ltmp], [lsum])
    p.op("act", lambda e: e.activation(out=lsum.t[:], in_=lsum.t[:], func=AF.Exp), [lsum], [lsum])
    p.op("dve", lambda e: e.tensor_tensor(out=neglam.t[:], in0=lsum.t[:, 1:2], in1=lsum.t[:, 0:1], op=ALU.subtract), [lsum], [neglam])
    p.op("dve", lambda e: e.tensor_scalar(out=neglam.t[:], in0=neglam.t[:], scalar1=-float(lam_init), scalar2=None, op0=ALU.add), [neglam], [neglam])
    p.op("dve", lambda e: e.tensor_scalar(out=sub_s.t[:], in0=sub_s.t[:], scalar1=float(1.0 - lam_init), scalar2=None, op0=ALU.mult), [sub_s], [sub_s])
    p.op("dve", lambda e: e.tensor_scalar(out=qkw_s.t[:, 0:1], in0=qkw_s.t[:, 0:1], scalar1=float(HD ** -0.5), scalar2=None, op0=ALU.mult), [qkw_s], [qkw_s])
    aneg = p.sb("aneg", [128, NG * HPG], F32)
    p.op("act", lambda e: e.activation(out=aneg.t[:], in_=alog_s.t[:], func=AF.Exp), [alog_s], [aneg])
    p.op("dve", lambda e: e.tensor_scalar(out=aneg.t[:], in0=aneg.t[:], scalar1=-1.0, scalar2=None, op0=ALU.mult), [aneg], [aneg])

    PA = [p.ps("pa%d" % i, [128, 512]) for i in range(2)]
    PO = [p.ps("po%d" % i, [128, 512]) for i in range(4)]
    PT = p.ps("pt", [128, 512], BF16)
    PS = p.ps("psm", [128, 512])


    def load_h(t):
        b = hbuf[t % 2]
        t = t % NT
        nsp = 4 if DC >= 4 else 1
        cs = DC // nsp
        for i in range(nsp):
            p.dma("sp" if i % 2 == 0 else "act", lambda e, i=i: e.dma_start(out=b.t[:, i * cs:(i + 1) * cs, :], in_=hT.t[i * cs * 128:(i + 1) * cs * 128, t * TT:(t + 1) * TT].rearrange("(c p) n -> p c n", p=128)), hT, b)
        return b

    p.phase_begin()
    hbuf = [p.sb("hbuf%d" % i, [128, DC, TT], BF16) for i in range(2)]
    with_w = p.sb("wbig", [128, DC, 1024 + 16], BF16)
    nqk = 2 * NH * 2
    for c in range(DC):
        p.dma("pool", lambda e, c=c: e.dma_start(out=with_w.t[:, c, 0:nqk * 128], in_=wqk.t[c * 128:(c + 1) * 128, :]), wqk, with_w)
    sq = [p.sb("sq%d" % i, [128, TT], F32) for i in range(2)]
    rs = [p.sb("rs%d" % i, [128, TT], F32) for i in range(2)]
    ob = [p.sb("ob%d" % i, [128, TT], BF16) for i in range(2)]
    it = 0
    for t in range(NT):
        hb = load_h(t)
        for j in range(nqk):
            pa = PA[it % 2]; s_ = sq[it % 2]; r_ = rs[it % 2]; o_ = ob[it % 2]; it += 1
            for c in range(DC):
                p.op("pe", lambda e, pa=pa, c=c, j=j, hb=hb: e.matmul(pa.t[:, 0:TT], with_w.t[:, c, j * 128:(j + 1) * 128], hb.t[:, c, :], start=(c == 0), stop=(c == DC - 1)), [with_w, hb], [pa])
            p.op("act", lambda e, pa=pa, s_=s_: e.activation(out=s_.t[:], in_=pa.t[:, 0:TT], func=AF.Square), [pa], [s_])
            p.op("pe", lambda e, s_=s_: e.matmul(PS.t[:, 0:TT], ones_f.t[:], s_.t[:], start=True, stop=True), [ones_f, s_], [PS])
            p.op("act", lambda e, r_=r_: e.activation(out=r_.t[:], in_=PS.t[:, 0:TT], func=AF.Sqrt, scale=1.0 / HD, bias=EPS), [PS], [r_])
            p.op("dve", lambda e, r_=r_: e.reciprocal(out=r_.t[:], in_=r_.t[:]), [r_], [r_])
            wcol = 0 if j < 2 * NH else 1
            p.op("dve", lambda e, pa=pa, r_=r_, o_=o_, wcol=wcol: e.scalar_tensor_tensor(out=o_.t[:], in0=pa.t[:, 0:TT], scalar=qkw_s.t[:, wcol:wcol + 1], in1=r_.t[:], op0=ALU.mult, op1=ALU.mult), [pa, r_, qkw_s], [o_])
            dst = QT if j < 2 * NH else KT
            jj = j % (2 * NH)
            p.dma("sp", lambda e, o_=o_, dst=dst, jj=jj, t=t: e.dma_start(out=dst.t[jj, :, t * TT:(t + 1) * TT], in_=o_.t[:]), o_, dst)

    p.phase_end()
    p.phase_begin()
    hbuf = [p.sb("hbuf%d" % i, [128, DC, TT], BF16) for i in range(2)]
    with_w = p.sb("wbig", [128, DC, 1024 + 16], BF16)
    ob = [p.sb("ob%d" % i, [128, TT], BF16) for i in range(2)]
    for c in range(DC):
        p.dma("pool", lambda e, c=c: e.dma_start(out=with_w.t[:, c, 0:NXB], in_=wxbc.t[c * 128:(c + 1) * 128, :]), wxbc, with_w)
    cbuf = [p.sb("cbuf%d" % j, [128, CK - 1 + TT], F32) for j in range(NXC)]
    for j in range(NXC):
        p.op("dve", lambda e, j=j: e.memset(cbuf[j].t[:], 0.0), [], [cbuf[j]])
    acc = [p.sb("acc%d" % i, [128, TT], F32) for i in range(2)]
    for t in range(NT):
        hb = load_h(NT + t)
        for j in range(NXC):
            pa = PA[it % 2]; a_ = acc[it % 2]; o_ = ob[it % 2]; it += 1
            for c in range(DC):
                p.op("pe", lambda e, pa=pa, c=c, j=j, hb=hb: e.matmul(pa.t[:, 0:TT], with_w.t[:, c, j * 128:(j + 1) * 128], hb.t[:, c, :], start=(c == 0), stop=(c == DC - 1)), [with_w, hb], [pa])
            cbj = cbuf[j]
            p.op("act", lambda e, pa=pa, cbj=cbj: e.copy(out=cbj.t[:, CK - 1:], in_=pa.t[:, 0:TT]), [pa], [cbj])
            p.op("dve", lambda e, a_=a_, cbj=cbj, j=j: e.tensor_scalar(out=a_.t[:], in0=cbj.t[:, 0:TT], scalar1=cw_s.t[:, j, 0:1], scalar2=cb_s.t[:, j:j + 1], op0=ALU.mult, op1=ALU.add), [cbj, cw_s, cb_s], [a_])
            for tap in range(1, CK):
                p.op("dve", lambda e, a_=a_, cbj=cbj, j=j, tap=tap: e.scalar_tensor_tensor(out=a_.t[:], in0=cbj.t[:, tap:tap + TT], scalar=cw_s.t[:, j, tap:tap + 1], in1=a_.t[:], op0=ALU.mult, op1=ALU.add), [cbj, cw_s, a_], [a_])
            p.op("act", lambda e, a_=a_, o_=o_: e.activation(out=o_.t[:], in_=a_.t[:], func=AF.Silu), [a_], [o_])
            p.dma("sp", lambda e, o_=o_, j=j, t=t: e.dma_start(out=XBC.t[j, :, t * TT:(t + 1) * TT], in_=o_.t[:]), o_, XBC)
            p.op("dve", lambda e, cbj=cbj: e.tensor_copy(out=cbj.t[:, 0:CK - 1], in_=cbj.t[:, TT:TT + CK - 1]), [cbj], [cbj])

    p.phase_end()
    p.phase_begin()
    hbuf = [p.sb("hbuf%d" % i, [128, DC, TT], BF16) for i in range(2)]
    with_w = p.sb("wbig", [128, DC, 1024 + 16], BF16)
    NTK = NH * VD + NG * HPG * PD + NG * HPG
    for c in range(DC):
        p.dma("pool", lambda e, c=c: e.dma_start(out=with_w.t[:, c, 0:NTK], in_=wtok.t[c * 128:(c + 1) * 128, :]), wtok, with_w)
    vo = [p.sb("vo%d" % i, [128, NH, VD + 1], BF16) for i in range(2)]
    zo = [p.sb("zo%d" % i, [128, NG * HPG * PD], BF16) for i in range(2)]
    do = [p.sb("do%d" % i, [128, NG * HPG], F32) for i in range(2)]
    for i in range(2):
        p.op("dve", lambda e, i=i: e.memset(vo[i].t[:], 1.0), [], [vo[i]])
    zoff = NH * VD; doff = zoff + NG * HPG * PD
    for t in range(NT):
        hb = load_h(2 * NT + t)
        for s in range(TT // 128):
            blk = t * (TT // 128) + s
            pv = PA[0]; pz = PA[1]; pd = PS
            v_ = vo[blk % 2]; z_ = zo[blk % 2]; d_ = do[blk % 2]
            for (pp, lo, n) in ((pv, 0, NH * VD), (pz, zoff, NG * HPG * PD), (pd, doff, NG * HPG)):
                for c in range(DC):
                    p.op("pe", lambda e, pp=pp, c=c, lo=lo, n=n, hb=hb, s=s: e.matmul(pp.t[:, 0:n], hb.t[:, c, s * 128:(s + 1) * 128], with_w.t[:, c, lo:lo + n], start=(c == 0), stop=(c == DC - 1)), [with_w, hb], [pp])
            p.op("act", lambda e, v_=v_, pv=pv: e.copy(out=v_.t[:, :, 0:VD], in_=pv.t[:, 0:NH * VD].rearrange("p (h e) -> p h e", h=NH)), [pv], [v_])
            p.dma("sp", lambda e, v_=v_, blk=blk: e.dma_start(out=VE.t[blk * 128:(blk + 1) * 128, :, :], in_=v_.t[:]), v_, VE)
            p.op("act", lambda e, z_=z_, pz=pz: e.activation(out=z_.t[:], in_=pz.t[:, 0:NG * HPG * PD], func=AF.Silu), [pz], [z_])
            p.dma("sp", lambda e, z_=z_, blk=blk: e.dma_start(out=SZ.t[blk * 128:(blk + 1) * 128, :], in_=z_.t[:]), z_, SZ)
            p.op("dve", lambda e, d_=d_, pd=pd: e.tensor_tensor(out=d_.t[:], in0=pd.t[:, 0:NG * HPG], in1=dtb_s.t[:], op=ALU.add), [pd, dtb_s], [d_])
            p.op("act", lambda e, d_=d_: e.activation(out=d_.t[:], in_=d_.t[:], func=AF.Exp), [d_], [d_])
            p.op("act", lambda e, d_=d_: e.activation(out=d_.t[:], in_=d_.t[:], func=AF.Ln, bias=1.0), [d_], [d_])
            p.dma("sp", lambda e, d_=d_, blk=blk: e.dma_start(out=DT.t[blk * 128:(blk + 1) * 128, :], in_=d_.t[:]), d_, DT)

    p.phase_end()
    p.phase_begin()
    QTILE = 256 if S >= 256 else S
    NQS = QTILE // 128
    kts = p.sb("kts", [128, 2, S], BF16)
    ves = p.sb("ves", [128, NB, VD + 1], BF16)
    qts = [p.sb("qts%d" % i, [128, 2, QTILE], BF16) for i in range(2)]
    pts = [p.sb("pts%d" % i, [128, QTILE], BF16) for i in range(3)]
    o1 = [p.sb("o1_%d" % i, [128, VD], F32) for i in range(2)]
    rr = [p.sb("rr%d" % i, [128, 4], F32) for i in range(2)]
    osq = p.sb("osq", [128, VD], F32)
    oo = [p.sb("oo%d" % i, [128, VD], BF16) for i in range(2)]
    ipt = 0; iq = 0; io = 0
    for hd in range(NH):
        p.dma("sp", lambda e, hd=hd: e.dma_start(out=kts.t[:], in_=KT.t[2 * hd:2 * hd + 2, :, :].rearrange("c p n -> p c n")), KT, kts)
        for b0 in range(0, NB, 4):
            b1 = min(NB, b0 + 4)
            p.dma("act", lambda e, hd=hd, b0=b0, b1=b1: e.dma_start(out=ves.t[:, b0:b1, :], in_=VE.t[b0 * 128:b1 * 128, hd, :].rearrange("(b p) e -> p b e", p=128)), VE, ves)
        for qi in range(S // QTILE):
            qt = qts[iq % 2]; iq += 1
            p.dma("sp", lambda e, qt=qt, hd=hd, qi=qi: e.dma_start(out=qt.t[:], in_=QT.t[2 * hd:2 * hd + 2, :, qi * QTILE:(qi + 1) * QTILE].rearrange("c p n -> p c n")), QT, qt)
            nkb = NQS * (qi + 1)
            for comp in range(2):
                for kb in range(nkb):
                    pa = PA[ipt % 2]; pt = pts[ipt % 3]; ipt += 1
                    p.op("pe", lambda e, pa=pa, comp=comp, kb=kb, qt=qt: e.matmul(pa.t[:, 0:QTILE], kts.t[:, comp, kb * 128:(kb + 1) * 128], qt.t[:, comp, :], start=True, stop=True), [kts, qt], [pa])
                    p.op("act", lambda e, pa=pa, pt=pt: e.activation(out=pt.t[:], in_=pa.t[:, 0:QTILE], func=AF.Exp), [pa], [pt])
                    dq = kb - NQS * qi
                    if dq >= 0:
                        p.op("dve", lambda e, pt=pt, dq=dq: e.tensor_tensor(out=pt.t[:, dq * 128:(dq + 1) * 128], in0=pt.t[:, dq * 128:(dq + 1) * 128], in1=mincl_b.t[:], op=ALU.mult), [pt, mincl_b], [pt])
                    for qs in range(NQS):
                        last = NQS * qi + qs
                        if kb > last:
                            continue
                        po = PO[comp * 2 + qs]
                        p.op("pe", lambda e, po=po, pt=pt, qs=qs, kb=kb, last=last: e.matmul(po.t[:, 0:VD + 1], pt.t[:, qs * 128:(qs + 1) * 128], ves.t[:, kb, :], start=(kb == 0), stop=(kb == last)), [pt, ves], [po])
            for qs in range(NQS):
                r_ = rr[io % 2]; o_ = o1[io % 2]; ob_ = oo[io % 2]; io += 1
                pa_, pb_ = PO[qs], PO[2 + qs]
                p.op("dve", lambda e, r_=r_, pa_=pa_: e.reciprocal(out=r_.t[:, 0:1], in_=pa_.t[:, VD:VD + 1]), [pa_], [r_])
                p.op("dve", lambda e, r_=r_, pb_=pb_: e.reciprocal(out=r_.t[:, 1:2], in_=pb_.t[:, VD:VD + 1]), [pb_], [r_])
                p.op("dve", lambda e, r_=r_: e.tensor_tensor(out=r_.t[:, 1:2], in0=r_.t[:, 1:2], in1=neglam.t[:], op=ALU.mult), [r_, neglam], [r_])
                p.op("dve", lambda e, r_=r_, o_=o_, pa_=pa_: e.tensor_scalar(out=o_.t[:], in0=pa_.t[:, 0:VD], scalar1=r_.t[:, 0:1], scalar2=None, op0=ALU.mult), [pa_, r_], [o_])
                p.op("dve", lambda e, r_=r_, o_=o_, pb_=pb_: e.scalar_tensor_tensor(out=o_.t[:], in0=pb_.t[:, 0:VD], scalar=r_.t[:, 1:2], in1=o_.t[:], op0=ALU.mult, op1=ALU.add), [pb_, r_, o_], [o_])
                p.op("act", lambda e, o_=o_: e.activation(out=osq.t[:], in_=o_.t[:], func=AF.Square), [o_], [osq])
                p.op("dve", lambda e, r_=r_: e.tensor_reduce(out=r_.t[:, 2:3], in_=osq.t[:], axis=AX.X, op=ALU.add), [osq], [r_])
                p.op("act", lambda e, r_=r_: e.activation(out=r_.t[:, 2:3], in_=r_.t[:, 2:3], func=AF.Sqrt, scale=1.0 / VD, bias=EPS), [r_], [r_])
                p.op("dve", lambda e, r_=r_: e.reciprocal(out=r_.t[:, 2:3], in_=r_.t[:, 2:3]), [r_], [r_])
                p.op("dve", lambda e, r_=r_, o_=o_, ob_=ob_: e.scalar_tensor_tensor(out=ob_.t[:], in0=o_.t[:], scalar=r_.t[:, 2:3], in1=sub_s.t[:], op0=ALU.mult, op1=ALU.mult), [o_, r_, sub_s], [ob_])
                tok0 = qi * QTILE + qs * 128
                p.dma("sp", lambda e, ob_=ob_, tok0=tok0, hd=hd: e.dma_start(out=mix.t[tok0:tok0 + 128, hd * VD:(hd + 1) * VD], in_=ob_.t[:]), ob_, mix)

    p.phase_end()
    p.phase_begin()
    GW = HPG * PD
    for g in range(NG):
        stf = p.sb("stf%d" % g, [128, GW], F32); stb = p.sb("stb%d" % g, [128, GW], BF16)
        p.op("dve", lambda e, stf=stf: e.memset(stf.t[:], 0.0), [], [stf])
        p.op("dve", lambda e, stb=stb: e.memset(stb.t[:], 0.0), [], [stb])
        xch = [2 * g, 2 * g + 1]; bch = NG * 2 + g; cch = NG * 2 + NG + g
        nbuf = 2
        xt_ = [p.sb("xt%d_%d" % (g, i), [128, 2, 128], BF16) for i in range(nbuf)]
        bt_ = [p.sb("bt%d_%d" % (g, i), [128, 128], BF16) for i in range(nbuf)]
        ct_ = [p.sb("ct%d_%d" % (g, i), [128, 128], BF16) for i in range(nbuf)]
        dt_ = [p.sb("dt%d_%d" % (g, i), [128, HPG], F32) for i in range(nbuf)]
        sz_ = [p.sb("sz%d_%d" % (g, i), [128, GW], BF16) for i in range(nbuf)]
        xm = [p.sb("xm%d_%d" % (g, i), [128, GW], BF16) for i in range(nbuf)]
        bm = [p.sb("bm%d_%d" % (g, i), [128, 128], BF16) for i in range(nbuf)]
        dta = [p.sb("dta%d_%d" % (g, i), [128, HPG], F32) for i in range(nbuf)]
        sm = [p.sb("sm%d_%d" % (g, i), [128, 4 * HPG], F32) for i in range(nbuf)]
        cbm = [p.sb("cbm%d_%d" % (g, i), [128, 128], F32) for i in range(nbuf)]
        Rb = [p.sb("R%d_%d" % (g, i), [128, 128], F32) for i in range(2)]
        dec = [p.sb("dec%d_%d" % (g, i), [128, 128], F32) for i in range(2)]
        wt = [p.sb("wt%d_%d" % (g, i), [128, 128], BF16) for i in range(2)]
        eab = [p.sb("eab%d_%d" % (g, i), [128, 128], F32) for i in range(2)]
        cpr = [p.sb("cpr%d_%d" % (g, i), [128, 128], BF16) for i in range(2)]
        yf = [p.sb("yf%d_%d" % (g, i), [128, GW], F32) for i in range(2)]
        ysq = p.sb("ysq%d" % g, [128, GW], F32)
        yr = [p.sb("yr%d_%d" % (g, i), [128, 2], F32) for i in range(2)]
        yo = [p.sb("yo%d_%d" % (g, i), [128, GW], BF16) for i in range(2)]
        xw = [p.sb("xw%d_%d" % (g, i), [128, GW], BF16) for i in range(2)]
        ih = 0
        for c in range(NB):
            i = c % nbuf
            X_, B_, C_, D_, Z_ = xt_[i], bt_[i], ct_[i], dt_[i], sz_[i]
            sl = slice(c * 128, (c + 1) * 128)
            p.dma("sp", lambda e, X_=X_, sl=sl: e.dma_start(out=X_.t[:], in_=XBC.t[xch[0]:xch[0] + 2, :, sl].rearrange("c p n -> p c n")), XBC, X_)
            p.dma("sp", lambda e, B_=B_, sl=sl: e.dma_start(out=B_.t[:], in_=XBC.t[bch, :, sl]), XBC, B_)
            p.dma("act", lambda e, C_=C_, sl=sl: e.dma_start(out=C_.t[:], in_=XBC.t[cch, :, sl]), XBC, C_)
            p.dma("act", lambda e, D_=D_, sl=sl: e.dma_start(out=D_.t[:], in_=DT.t[sl, g * HPG:(g + 1) * HPG]), DT, D_)
            p.dma("act", lambda e, Z_=Z_, sl=sl: e.dma_start(out=Z_.t[:], in_=SZ.t[sl, g * GW:(g + 1) * GW]), SZ, Z_)
            XM, BM = xm[i], bm[i]
            for k in range(2):
                p.op("pe", lambda e, X_=X_, k=k: e.transpose(PT.t[:, k * 128:(k + 1) * 128], X_.t[:, k, :], ident_b.t[:]), [X_, ident_b], [PT])
            p.op("pe", lambda e, B_=B_: e.transpose(PT.t[:, 256:384], B_.t[:], ident_b.t[:]), [B_, ident_b], [PT])
            p.op("act", lambda e, XM=XM: e.copy(out=XM.t[:], in_=PT.t[:, 0:256]), [PT], [XM])
            p.op("act", lambda e, BM=BM: e.copy(out=BM.t[:], in_=PT.t[:, 256:384]), [PT], [BM])
            DA = dta[i]; SM = sm[i]
            p.op("dve", lambda e, DA=DA, D_=D_: e.tensor_tensor(out=DA.t[:], in0=D_.t[:], in1=aneg.t[:, g * HPG:(g + 1) * HPG], op=ALU.mult), [D_, aneg], [DA])
            p.op("pe", lambda e, DA=DA: e.matmul(PS.t[:, 0:HPG], mincl_f.t[:], DA.t[:], start=True, stop=True), [mincl_f, DA], [PS])
            p.op("pe", lambda e, DA=DA: e.matmul(PS.t[:, 8:8 + HPG], ones_f.t[:], DA.t[:], start=True, stop=True), [ones_f, DA], [PS])
            p.op("act", lambda e, SM=SM: e.copy(out=SM.t[:, 0:HPG], in_=PS.t[:, 0:HPG]), [PS], [SM])
            p.op("act", lambda e, SM=SM: e.activation(out=SM.t[:, HPG:2 * HPG], in_=PS.t[:, 8:8 + HPG], func=AF.Exp), [PS], [SM])
            p.op("dve", lambda e, SM=SM: e.tensor_tensor(out=SM.t[:, 2 * HPG:3 * HPG], in0=PS.t[:, 8:8 + HPG], in1=SM.t[:, 0:HPG], op=ALU.subtract), [PS, SM], [SM])
            p.op("act", lambda e, SM=SM: e.activation(out=SM.t[:, 2 * HPG:3 * HPG], in_=SM.t[:, 2 * HPG:3 * HPG], func=AF.Exp), [SM], [SM])
            p.op("dve", lambda e, SM=SM, D_=D_: e.tensor_tensor(out=SM.t[:, 2 * HPG:3 * HPG], in0=SM.t[:, 2 * HPG:3 * HPG], in1=D_.t[:], op=ALU.mult), [SM, D_], [SM])
            CB = cbm[i]
            p.op("pe", lambda e, B_=B_, C_=C_: e.matmul(PA[0].t[:, 0:128], B_.t[:], C_.t[:], start=True, stop=True), [B_, C_], [PA[0]])
            p.op("dve", lambda e, CB=CB: e.tensor_tensor(out=CB.t[:], in0=PA[0].t[:, 0:128], in1=mincl_f.t[:], op=ALU.mult), [PA[0], mincl_f], [CB])
            PY = PO[c % 2]
            for h in range(HPG):
                R_ = Rb[ih % 2]; DE = dec[ih % 2]; WT = wt[ih % 2]; EA = eab[ih % 2]; CP = cpr[ih % 2]; ih += 1
                pseg = PA[1]; pea = PO[2]
                p.op("dve", lambda e, R_=R_, DA=DA, h=h: e.tensor_scalar(out=R_.t[:], in0=mincl_f.t[:], scalar1=DA.t[:, h:h + 1], scalar2=None, op0=ALU.mult), [mincl_f, DA], [R_])
                p.op("pe", lambda e, R_=R_, pseg=pseg: e.matmul(pseg.t[:, 0:128], ugt_f.t[:], R_.t[:], start=True, stop=True), [ugt_f, R_], [pseg])
                p.op("pe", lambda e, R_=R_, pea=pea: e.matmul(pea.t[:, 0:128], ones_f.t[:], R_.t[:], start=True, stop=True), [ones_f, R_], [pea])
                p.op("act", lambda e, DE=DE, pseg=pseg: e.activation(out=DE.t[:], in_=pseg.t[:, 0:128], func=AF.Exp), [pseg], [DE])
                p.op("act", lambda e, EA=EA, pea=pea: e.activation(out=EA.t[:], in_=pea.t[:, 0:128], func=AF.Exp), [pea], [EA])
                p.op("dve", lambda e, WT=WT, DE=DE, D_=D_, CB=CB, h=h: e.scalar_tensor_tensor(out=WT.t[:], in0=DE.t[:], scalar=D_.t[:, h:h + 1], in1=CB.t[:], op0=ALU.mult, op1=ALU.mult), [DE, D_, CB], [WT])
                p.op("dve", lambda e, CP=CP, EA=EA, C_=C_: e.tensor_tensor(out=CP.t[:], in0=EA.t[:], in1=C_.t[:], op=ALU.mult), [EA, C_], [CP])
                p.op("pe", lambda e, PY=PY, WT=WT, XM=XM, h=h: e.matmul(PY.t[:, h * PD:(h + 1) * PD], WT.t[:], XM.t[:, h * PD:(h + 1) * PD], start=True, stop=False), [WT, XM], [PY])
                p.op("pe", lambda e, PY=PY, CP=CP, h=h: e.matmul(PY.t[:, h * PD:(h + 1) * PD], CP.t[:], stb.t[:, h * PD:(h + 1) * PD], start=False, stop=True), [CP, stb, PY], [PY])
            YF = yf[c % 2]; YR = yr[c % 2]; YO = yo[c % 2]
            for h in range(HPG):
                hs = slice(h * PD, (h + 1) * PD)
                p.op("dve", lambda e, YF=YF, XM=XM, PY=PY, hs=hs, h=h: e.scalar_tensor_tensor(out=YF.t[:, hs], in0=XM.t[:, hs], scalar=dsk_s.t[:, g * HPG + h:g * HPG + h + 1], in1=PY.t[:, hs], op0=ALU.mult, op1=ALU.add), [XM, dsk_s, PY], [YF])
            p.op("dve", lambda e, YF=YF, Z_=Z_: e.tensor_tensor(out=YF.t[:], in0=YF.t[:], in1=Z_.t[:], op=ALU.mult), [YF, Z_], [YF])
            p.op("act", lambda e, YF=YF: e.activation(out=ysq.t[:], in_=YF.t[:], func=AF.Square), [YF], [ysq])
            p.op("dve", lambda e, YR=YR: e.tensor_reduce(out=YR.t[:, 0:1], in_=ysq.t[:], axis=AX.X, op=ALU.add), [ysq], [YR])
            p.op("act", lambda e, YR=YR: e.activation(out=YR.t[:, 0:1], in_=YR.t[:, 0:1], func=AF.Sqrt, scale=1.0 / GW, bias=EPS), [YR], [YR])
            p.op("dve", lambda e, YR=YR: e.reciprocal(out=YR.t[:, 0:1], in_=YR.t[:, 0:1]), [YR], [YR])
            p.op("dve", lambda e, YO=YO, YF=YF, YR=YR: e.scalar_tensor_tensor(out=YO.t[:], in0=YF.t[:], scalar=YR.t[:, 0:1], in1=snw_s.t[:, g * GW:(g + 1) * GW], op0=ALU.mult, op1=ALU.mult), [YF, YR, snw_s], [YO])
            p.dma("sp", lambda e, YO=YO, sl=sl: e.dma_start(out=mix.t[sl, NH * VD + g * GW:NH * VD + (g + 1) * GW], in_=YO.t[:]), YO, mix)
            XW = xw[c % 2]
            for h in range(HPG):
                hs = slice(h * PD, (h + 1) * PD)
                p.op("dve", lambda e, XW=XW, XM=XM, SM=SM, hs=hs, h=h: e.tensor_scalar(out=XW.t[:, hs], in0=XM.t[:, hs], scalar1=SM.t[:, 2 * HPG + h:2 * HPG + h + 1], scalar2=None, op0=ALU.mult), [XM, SM], [XW])
            pst = PO[3]
            p.op("pe", lambda e, BM=BM, XW=XW: e.matmul(pst.t[:, 0:GW], BM.t[:], XW.t[:], start=True, stop=True), [BM, XW], [pst])
            for h in range(HPG):
                hs = slice(h * PD, (h + 1) * PD)
                p.op("dve", lambda e, SM=SM, hs=hs, h=h: e.scalar_tensor_tensor(out=stf.t[:, hs], in0=stf.t[:, hs], scalar=SM.t[:, HPG + h:HPG + h + 1], in1=pst.t[:, hs], op0=ALU.mult, op1=ALU.add), [stf, SM, pst], [stf])
            p.op("act", lambda e: e.copy(out=stb.t[:], in_=stf.t[:]), [stf], [stb])

    p.phase_end()
    p.finish()
    return nc


import numpy as np

EPS = 1e-6
NE = 8


def build_tok(nc, D, T, DMIX=0, FF=0, do_wout=False, do_norm=False, do_router=False, do_ffn=False,
              ffn_mode="res", do_next=False, h_in=False):
    p = Prog(nc)
    DC = D // 128
    TT = 512 if T >= 512 else T
    NT = T // TT
    din = lambda name, shape, dt=F32: p.dram(name, shape, dt, kind="ExternalInput")
    dout = lambda name, shape, dt=F32: p.dram(name, shape, dt, kind="ExternalOutput")
    cones = din("c_ones", [128, 128])
    if do_wout or do_norm:
        xT = din("xT", [D, T])
    if do_wout:
        MC = DMIX // 128
        mixT = din("mixT", [DMIX, T], BF16); w_out = din("w_out", [DMIX, D])
        WoB = p.dram("WoB", [DC, 128, MC, 128], BF16)
    if do_norm:
        n2w = din("n2w", [128, DC])
    if h_in:
        hTin = din("hT", [D, T], BF16)
    if do_router:
        rw = din("rw", [D, NE]); comb_out = dout("comb_out", [T, NE])
    if do_ffn:
        NJ = FF // 128
        wg = din("wg", [D, FF]); wu = din("wu", [D, FF]); wd = din("wd", [FF, D])
        WgB = p.dram("WgB", [NJ, 128, DC, 128], BF16); WuB = p.dram("WuB", [NJ, 128, DC, 128], BF16)
        WdB = p.dram("WdB", [DC, 128, NJ, 128], BF16)
        yT = dout("yT", [D, T])
        if ffn_mode == "comb":
            combb = din("comb", [128, T])
    if do_next:
        n1w = din("n1w", [128, DC]); hnT = dout("hnT", [D, T], BF16)
    need_x1_out = (do_wout or do_norm) and not (do_ffn and ffn_mode == "res")
    if do_wout:
        x1T = dout("x1T", [D, T]) if need_x1_out else p.dram("x1T", [D, T], F32)
    if do_norm and not do_ffn:
        hT_out = dout("hT_out", [D, T], BF16)

    ones_f = p.sb("ones_f", [128, 128], F32)
    p.dma("sp", lambda e: e.dma_start(out=ones_f.t[:], in_=cones.t[:]), cones, ones_f)
    if do_norm:
        n2w_s = p.sb("n2w_s", [128, DC], F32)
        p.dma("sp", lambda e: e.dma_start(out=n2w_s.t[:], in_=n2w.t[:]), n2w, n2w_s)
    if do_next:
        n1w_s = p.sb("n1w_s", [128, DC], F32)
        p.dma("sp", lambda e: e.dma_start(out=n1w_s.t[:], in_=n1w.t[:]), n1w, n1w_s)
    if do_router:
        rw_s = p.sb("rw_s", [128, DC, NE], F32)
        p.dma("sp", lambda e: e.dma_start(out=rw_s.t[:], in_=rw.t[:, :].rearrange("(c p) n -> p c n", p=128)), rw, rw_s)

    PW = [p.ps("pw%d" % i, [128, 512]) for i in range(2)]
    PG = [p.ps("pg%d" % i, [128, 512]) for i in range(2)]
    PU = [p.ps("pu%d" % i, [128, 512]) for i in range(2)]
    PSS = p.ps("pss", [128, 512])
    PR = p.ps("pr", [128, 512])

    qi = [0]
    def castq():
        qi[0] += 1
        return "pool"
    if do_wout:
        for fo in range(DC):
            for c0 in range(0, MC, 8):
                c1 = min(MC, c0 + 8)
                p.dma(castq(), lambda e, fo=fo, c0=c0, c1=c1: e.dma_start(out=WoB.t[fo, :, c0:c1, :], in_=w_out.t[c0 * 128:c1 * 128, fo * 128:(fo + 1) * 128].rearrange("(c p) n -> p c n", p=128)), w_out, WoB)
    if do_ffn:
        for j in range(NJ):
            for c0 in range(0, DC, 8):
                c1 = min(DC, c0 + 8)
                for (src, dst) in ((wg, WgB), (wu, WuB)):
                    p.dma(castq(), lambda e, j=j, c0=c0, c1=c1, src=src, dst=dst: e.dma_start(out=dst.t[j, :, c0:c1, :], in_=src.t[c0 * 128:c1 * 128, j * 128:(j + 1) * 128].rearrange("(c p) n -> p c n", p=128)), src, dst)
        for fo in range(DC):
            for j0 in range(0, NJ, 8):
                j1 = min(NJ, j0 + 8)
                p.dma(castq(), lambda e, fo=fo, j0=j0, j1=j1: e.dma_start(out=WdB.t[fo, :, j0:j1, :], in_=wd.t[j0 * 128:j1 * 128, fo * 128:(fo + 1) * 128].rearrange("(c p) n -> p c n", p=128)), wd, WdB)
    p.barrier()

    ACH = max(DC, (DMIX // 128) if do_wout else 0)
    A = p.sb("A", [128, ACH, TT], BF16)
    if do_ffn:
        HJ = (NJ + 1) // 2 if NJ > 44 else NJ
        WSZ = max(DC, HJ, (DMIX // 128) if do_wout else 0) * 128
        actT = p.sb("actT", [128, NJ, TT], BF16)
    else:
        WSZ = max(DC, (DMIX // 128) if do_wout else 1) * 128
    NWB = 3
    wb = [p.sb("wb%d" % i, [128, WSZ], BF16) for i in range(NWB)]
    xc = [p.sb("xc%d" % i, [128, TT], F32) for i in range(3)]
    sq = [p.sb("sq%d" % i, [128, TT], F32) for i in range(2)]
    sg = [p.sb("sg%d" % i, [128, TT], F32) for i in range(2)]
    hb16 = [p.sb("hb16_%d" % i, [128, TT], BF16) for i in range(2)]
    rstd = p.sb("rstd", [128, TT], F32)
    if ffn_mode == "comb" and do_ffn:
        cmb = p.sb("cmb", [128, TT], F32)
    if do_router:
        hf = [p.sb("hf%d" % i, [128, TT], F32) for i in range(2)]
        lg = p.sb("lg", [128, (TT // 128) * NE], F32)
        rt = [p.sb("rt%d" % i, [128, NE], F32) for i in range(4)]
        rm = p.sb("rm", [128, 4], F32)
    cnt = {"w": 0, "x": 0, "s": 0, "g": 0, "h": 0, "q": 0}

    def nxt(k, lst):
        b = lst[cnt[k] % len(lst)]; cnt[k] += 1
        return b

    def dq():
        cnt["q"] += 1
        return "sp" if cnt["q"] % 2 else "act"

    def rms_from_pss(dst):
        p.op("act", lambda e: e.activation(out=dst.t[:], in_=PSS.t[:, 0:TT], func=AF.Sqrt, scale=1.0 / D, bias=EPS), [PSS], [dst])
        p.op("dve", lambda e: e.reciprocal(out=dst.t[:], in_=dst.t[:]), [dst], [dst])

    def accum_sumsq(src, first, last):
        s_ = nxt("s", sq)
        p.op("act", lambda e: e.activation(out=s_.t[:], in_=src.t[:], func=AF.Square), [src], [s_])
        p.op("pe", lambda e: e.matmul(PSS.t[:, 0:TT], ones_f.t[:], s_.t[:], start=first, stop=last), [ones_f, s_], [PSS])

    for t in range(NT):
        ts = slice(t * TT, (t + 1) * TT)
        if do_wout:
            for c0 in range(0, MC, 8):
                c1 = min(MC, c0 + 8)
                p.dma(dq(), lambda e, c0=c0, c1=c1: e.dma_start(out=A.t[:, c0:c1, :], in_=mixT.t[c0 * 128:c1 * 128, ts].rearrange("(c p) n -> p c n", p=128)), mixT, A)
            for fo in range(DC):
                w_ = nxt("w", wb); pw = PW[fo % 2]; x_ = nxt("x", xc)
                p.dma(dq(), lambda e: e.dma_start(out=w_.t[:, 0:MC * 128], in_=WoB.t[fo, :, :, :].rearrange("p c n -> p (c n)")), WoB, w_)
                p.dma(dq(), lambda e: e.dma_start(out=x_.t[:], in_=xT.t[fo * 128:(fo + 1) * 128, ts]), xT, x_)
                for c in range(MC):
                    p.op("pe", lambda e, c=c: e.matmul(pw.t[:, 0:TT], w_.t[:, c * 128:(c + 1) * 128], A.t[:, c, :], start=(c == 0), stop=(c == MC - 1)), [w_, A], [pw])
                p.op("dve", lambda e: e.tensor_tensor(out=x_.t[:], in0=pw.t[:, 0:TT], in1=x_.t[:], op=ALU.add), [pw, x_], [x_])
                p.dma(dq(), lambda e: e.dma_start(out=x1T.t[fo * 128:(fo + 1) * 128, ts], in_=x_.t[:]), x_, x1T)
                if do_norm:
                    accum_sumsq(x_, fo == 0, fo == DC - 1)
        elif do_norm:
            for fo in range(DC):
                x_ = nxt("x", xc)
                p.dma(dq(), lambda e: e.dma_start(out=x_.t[:], in_=xT.t[fo * 128:(fo + 1) * 128, ts]), xT, x_)
                accum_sumsq(x_, fo == 0, fo == DC - 1)
        if do_norm:
            rms_from_pss(rstd)
            srcT = x1T if do_wout else xT
            for c in range(DC):
                x_ = nxt("x", xc)
                p.dma(dq(), lambda e: e.dma_start(out=x_.t[:], in_=srcT.t[c * 128:(c + 1) * 128, ts]), srcT, x_)
                if do_router:
                    h_ = nxt("h", hf)
                    p.op("dve", lambda e: e.scalar_tensor_tensor(out=h_.t[:], in0=x_.t[:], scalar=n2w_s.t[:, c:c + 1], in1=rstd.t[:], op0=ALU.mult, op1=ALU.mult), [x_, n2w_s, rstd], [h_])
                    p.op("act", lambda e: e.copy(out=A.t[:, c, :], in_=h_.t[:]), [h_], [A])
                    for s in range(TT // 128):
                        prs = [PR, PG[0], PG[1], PU[0]][s]
                        p.op("pe", lambda e, s=s, prs=prs: e.matmul(prs.t[:, 0:NE], h_.t[:, s * 128:(s + 1) * 128], rw_s.t[:, c, :], start=(c == 0), stop=(c == DC - 1)), [h_, rw_s], [prs])
                else:
                    p.op("dve", lambda e: e.scalar_tensor_tensor(out=A.t[:, c, :], in0=x_.t[:], scalar=n2w_s.t[:, c:c + 1], in1=rstd.t[:], op0=ALU.mult, op1=ALU.mult), [x_, n2w_s, rstd], [A])
                if not do_ffn:
                    hb = nxt("g", hb16)
                    p.op("act", lambda e: e.copy(out=hb.t[:], in_=A.t[:, c, :]), [A], [hb])
                    p.dma(dq(), lambda e: e.dma_start(out=hT_out.t[c * 128:(c + 1) * 128, ts], in_=hb.t[:]), hb, hT_out)
            if do_router:
                for s in range(TT // 128):
                    prs = [PR, PG[0], PG[1], PU[0]][s]
                    p.op("act", lambda e, s=s, prs=prs: e.copy(out=lg.t[:, s * NE:(s + 1) * NE], in_=prs.t[:, 0:NE]), [prs], [lg])
                for s in range(TT // 128):
                    L = lg.t[:, s * NE:(s + 1) * NE]
                    e1, msk, e2, cb_ = rt
                    p.op("dve", lambda e: e.tensor_reduce(out=rm.t[:, 0:1], in_=L, axis=AX.X, op=ALU.max), [lg], [rm])
                    p.op("dve", lambda e: e.tensor_scalar(out=e1.t[:], in0=L, scalar1=rm.t[:, 0:1], scalar2=None, op0=ALU.is_equal), [lg, rm], [e1])
                    p.op("dve", lambda e: e.scalar_tensor_tensor(out=msk.t[:], in0=e1.t[:], scalar=-1e30, in1=L, op0=ALU.mult, op1=ALU.add), [e1, lg], [msk])
                    p.op("dve", lambda e: e.tensor_reduce(out=rm.t[:, 1:2], in_=msk.t[:], axis=AX.X, op=ALU.max), [msk], [rm])
                    p.op("dve", lambda e: e.tensor_scalar(out=e2.t[:], in0=msk.t[:], scalar1=rm.t[:, 1:2], scalar2=None, op0=ALU.is_equal), [msk, rm], [e2])
                    p.op("dve", lambda e: e.tensor_tensor(out=rm.t[:, 2:3], in0=rm.t[:, 0:1], in1=rm.t[:, 1:2], op=ALU.subtract), [rm], [rm])
                    p.op("act", lambda e: e.activation(out=rm.t[:, 3:4], in_=rm.t[:, 2:3], func=AF.Sigmoid, scale=-1.0), [rm], [rm])
                    p.op("act", lambda e: e.activation(out=rm.t[:, 2:3], in_=rm.t[:, 2:3], func=AF.Sigmoid), [rm], [rm])
                    p.op("dve", lambda e: e.tensor_scalar(out=cb_.t[:], in0=e1.t[:], scalar1=rm.t[:, 2:3], scalar2=None, op0=ALU.mult), [e1, rm], [cb_])
                    p.op("dve", lambda e: e.scalar_tensor_tensor(out=cb_.t[:], in0=e2.t[:], scalar=rm.t[:, 3:4], in1=cb_.t[:], op0=ALU.mult, op1=ALU.add), [e2, rm, cb_], [cb_])
                    p.dma(dq(), lambda e, s=s: e.dma_start(out=comb_out.t[t * TT + s * 128:t * TT + (s + 1) * 128, :], in_=cb_.t[:]), cb_, comb_out)
        if h_in:
            for c0 in range(0, DC, 8):
                c1 = min(DC, c0 + 8)
                p.dma(dq(), lambda e, c0=c0, c1=c1: e.dma_start(out=A.t[:, c0:c1, :], in_=hTin.t[c0 * 128:c1 * 128, ts].rearrange("(c p) n -> p c n", p=128)), hTin, A)
        if do_ffn:
            if ffn_mode == "comb":
                p.dma(dq(), lambda e: e.dma_start(out=cmb.t[:], in_=combb.t[:, ts]), combb, cmb)
            for j in range(NJ):
                wg_ = nxt("w", wb); wu_ = nxt("w", wb); pg = PG[j % 2]; pu = PU[j % 2]; s_ = nxt("g", sg)
                p.dma(dq(), lambda e: e.dma_start(out=wg_.t[:, 0:DC * 128], in_=WgB.t[j, :, :, :].rearrange("p c n -> p (c n)")), WgB, wg_)
                p.dma(dq(), lambda e: e.dma_start(out=wu_.t[:, 0:DC * 128], in_=WuB.t[j, :, :, :].rearrange("p c n -> p (c n)")), WuB, wu_)
                for c in range(DC):
                    p.op("pe", lambda e, c=c: e.matmul(pg.t[:, 0:TT], wg_.t[:, c * 128:(c + 1) * 128], A.t[:, c, :], start=(c == 0), stop=(c == DC - 1)), [wg_, A], [pg])
                for c in range(DC):
                    p.op("pe", lambda e, c=c: e.matmul(pu.t[:, 0:TT], wu_.t[:, c * 128:(c + 1) * 128], A.t[:, c, :], start=(c == 0), stop=(c == DC - 1)), [wu_, A], [pu])
                p.op("act", lambda e: e.activation(out=s_.t[:], in_=pg.t[:, 0:TT], func=AF.Silu), [pg], [s_])
                p.op("dve", lambda e: e.tensor_tensor(out=actT.t[:, j, :], in0=s_.t[:], in1=pu.t[:, 0:TT], op=ALU.mult), [s_, pu], [actT])
            for fo in range(DC):
                pw = PW[fo % 2]; x_ = nxt("x", xc)
                if ffn_mode == "res":
                    p.dma(dq(), lambda e: e.dma_start(out=x_.t[:], in_=x1T.t[fo * 128:(fo + 1) * 128, ts]), x1T, x_)
                for j0 in range(0, NJ, HJ):
                    j1 = min(NJ, j0 + HJ)
                    w_ = nxt("w", wb)
                    p.dma(dq(), lambda e: e.dma_start(out=w_.t[:, 0:(j1 - j0) * 128], in_=WdB.t[fo, :, j0:j1, :].rearrange("p c n -> p (c n)")), WdB, w_)
                    for j in range(j0, j1):
                        p.op("pe", lambda e, j=j: e.matmul(pw.t[:, 0:TT], w_.t[:, (j - j0) * 128:(j - j0 + 1) * 128], actT.t[:, j, :], start=(j == 0), stop=(j == NJ - 1)), [w_, actT], [pw])
                if ffn_mode == "res":
                    p.op("dve", lambda e: e.tensor_tensor(out=x_.t[:], in0=pw.t[:, 0:TT], in1=x_.t[:], op=ALU.add), [pw, x_], [x_])
                else:
                    p.op("dve", lambda e: e.tensor_tensor(out=x_.t[:], in0=pw.t[:, 0:TT], in1=cmb.t[:], op=ALU.mult), [pw, cmb], [x_])
                p.dma(dq(), lambda e: e.dma_start(out=yT.t[fo * 128:(fo + 1) * 128, ts], in_=x_.t[:]), x_, yT)
                if do_next:
                    accum_sumsq(x_, fo == 0, fo == DC - 1)
            if do_next:
                rms_from_pss(rstd)
                for c in range(DC):
                    x_ = nxt("x", xc); hb = nxt("h", hb16)
                    p.dma(dq(), lambda e: e.dma_start(out=x_.t[:], in_=yT.t[c * 128:(c + 1) * 128, ts]), yT, x_)
                    p.op("dve", lambda e: e.scalar_tensor_tensor(out=hb.t[:], in0=x_.t[:], scalar=n1w_s.t[:, c:c + 1], in1=rstd.t[:], op0=ALU.mult, op1=ALU.mult), [x_, n1w_s, rstd], [hb])
                    p.dma(dq(), lambda e: e.dma_start(out=hnT.t[c * 128:(c + 1) * 128, ts], in_=hb.t[:]), hb, hnT)
    p.barrier()
    p.finish()
    return nc


def build_reduce(nc, D, T, NP):
    p = Prog(nc)
    DC = D // 128
    x1T = p.dram("x1T", [D, T], F32, kind="ExternalInput")
    parts = p.dram("parts", [NP, D, T], F32, kind="ExternalInput")
    yT = p.dram("yT", [D, T], F32, kind="ExternalOutput")
    TT = 1024 if T >= 1024 else T
    acc = [p.sb("acc%d" % i, [128, TT], F32) for i in range(2)]
    pb = [p.sb("pb%d" % i, [128, TT], F32) for i in range(4)]
    k = 0; q = 0
    for c in range(DC):
        for t in range(T // TT):
            ts = slice(t * TT, (t + 1) * TT)
            a_ = acc[k % 2]; k += 1
            p.dma("sp", lambda e: e.dma_start(out=a_.t[:], in_=x1T.t[c * 128:(c + 1) * 128, ts]), x1T, a_)
            for i in range(NP):
                b_ = pb[q % 4]; q += 1
                p.dma("act" if i % 2 else "sp", lambda e, i=i: e.dma_start(out=b_.t[:], in_=parts.t[i, c * 128:(c + 1) * 128, ts]), parts, b_)
                p.op("dve" if i % 2 else "pool", lambda e: e.tensor_tensor(out=a_.t[:], in0=a_.t[:], in1=b_.t[:], op=ALU.add), [a_, b_], [a_])
            p.dma("sp", lambda e: e.dma_start(out=yT.t[c * 128:(c + 1) * 128, ts], in_=a_.t[:]), a_, yT)
    p.barrier()
    p.finish()
    return nc


import ml_dtypes as _mld
from concourse.bass_utils import run_bass_kernel_spmd

D_MODEL = 4096; SEQ = 8192; BATCH = 2; DEPTH = 2
D_FF = 11008; MOE_FF = 5632; N_EXP = 8
NCORE = 8
_cache = {}


def _lam_init(layer):
    import math
    return 0.8 - 0.6 * math.exp(-0.3 * layer)


def _nc(key, builder):
    if key not in _cache:
        nc = bass.Bass("TRN2", target_bir_lowering=False)
        builder(nc)
        _cache[key] = nc
    return _cache[key]


def _run(nc, in_maps):
    res = run_bass_kernel_spmd(nc, in_maps, core_ids=list(range(NCORE)))
    return res.results


def _lay(w):
    return np.ascontiguousarray(np.asarray(w, np.float32).reshape(-1, 128).T)


def _bc(v):
    v = np.asarray(v, np.float32)
    return np.ascontiguousarray(np.broadcast_to(v[None, :], (128, v.shape[0])))


def kernel(x, norm1_w, w_in, w_out, q_norm_w, k_norm_w, lambda_q1, lambda_k1, lambda_q2,
           lambda_k2, subln_w, conv_w, conv_b, dt_bias, a_log, d_skip, ssm_norm_w, norm2_w,
           ffn_w_gate, ffn_w_up, ffn_w_down, router_w, moe_w_gate, moe_w_up, moe_w_down):
    f32 = np.float32
    D = D_MODEL; T = BATCH * SEQ; TC = T // NCORE
    ones = np.ones((128, 128), f32)
    cst = consts_np()
    X = np.asarray(x, f32).reshape(T, D)
    xT = [np.ascontiguousarray(X[c * TC:(c + 1) * TC].T) for c in range(NCORE)]
    nc = _nc("n0", lambda nc: build_tok(nc, D, TC, do_norm=True))
    r = _run(nc, [dict(c_ones=ones, xT=xT[c], n2w=_lay(norm1_w[0])) for c in range(NCORE)])
    hT_all = np.concatenate([r[c]["hT_out"] for c in range(NCORE)], axis=1)
    AW = 2048; SW = 2048
    for layer in range(DEPTH):
        W = np.asarray(w_in[layer], f32)
        nc = _nc(("mix", layer), lambda nc: build_mixer(nc, D, SEQ, _lam_init(layer)))
        maps = []
        for c in range(NCORE):
            b, q = divmod(c, 4)
            o_q, o_k, o_v, o_z, o_x = 0, AW, 2 * AW, 3 * AW, 3 * AW + SW
            o_B = o_x + SW; o_C = o_B + 1024; o_dt = o_x + 4096
            wqk = np.concatenate([W[:, o_q + 512 * q:o_q + 512 * (q + 1)], W[:, o_k + 512 * q:o_k + 512 * (q + 1)]], 1)
            wxbc = np.concatenate([W[:, o_x + 512 * q:o_x + 512 * (q + 1)], W[:, o_B + 256 * q:o_B + 256 * (q + 1)],
                                   W[:, o_C + 256 * q:o_C + 256 * (q + 1)]], 1)
            wtok = np.concatenate([W[:, o_v + 512 * q:o_v + 512 * (q + 1)], W[:, o_z + 512 * q:o_z + 512 * (q + 1)],
                                   W[:, o_dt + 8 * q:o_dt + 8 * (q + 1)]], 1)
            cw = np.asarray(conv_w[layer], f32); cb = np.asarray(conv_b[layer], f32)
            csel = np.concatenate([np.arange(512 * q, 512 * (q + 1)), SW + np.arange(256 * q, 256 * (q + 1)),
                                   SW + 1024 + np.arange(256 * q, 256 * (q + 1))])
            cwq = cw[:, csel]; cbq = cb[csel]
            maps.append(dict(
                hT=np.ascontiguousarray(hT_all[:, b * SEQ:(b + 1) * SEQ]),
                wqk=np.ascontiguousarray(wqk), wxbc=np.ascontiguousarray(wxbc), wtok=np.ascontiguousarray(wtok),
                conv_w=np.ascontiguousarray(cwq.reshape(4, 8, 128).transpose(2, 1, 0)),
                conv_b=np.ascontiguousarray(cbq.reshape(8, 128).T),
                qkw=np.ascontiguousarray(np.stack([np.asarray(q_norm_w[layer], f32), np.asarray(k_norm_w[layer], f32)], 1)),
                lamv=_bc(np.concatenate([np.asarray(a[layer], f32) for a in (lambda_q1, lambda_k1, lambda_q2, lambda_k2)])),
                sublnw=_bc(subln_w[layer]), dtb=_bc(np.asarray(dt_bias[layer])[8 * q:8 * (q + 1)]),
                alog=_bc(np.asarray(a_log[layer])[8 * q:8 * (q + 1)]), dsk=_bc(np.asarray(d_skip[layer])[8 * q:8 * (q + 1)]),
                snw=_bc(np.asarray(ssm_norm_w[layer])[512 * q:512 * (q + 1)]), **cst))
        r = _run(nc, maps)
        mixfull = np.empty((T, D), _mld.bfloat16)
        for c in range(NCORE):
            b, q = divmod(c, 4)
            m = r[c]["mix"]
            mixfull[b * SEQ:(b + 1) * SEQ, 512 * q:512 * (q + 1)] = m[:, 0:512]
            mixfull[b * SEQ:(b + 1) * SEQ, AW + 512 * q:AW + 512 * (q + 1)] = m[:, 512:1024]
        mixT = [np.ascontiguousarray(mixfull[c * TC:(c + 1) * TC].T) for c in range(NCORE)]
        del mixfull, r
        if layer % 2 == 0:
            i = layer // 2
            has_next = layer + 1 < DEPTH
            nc = _nc(("c0", has_next), lambda nc: build_tok(nc, D, TC, DMIX=D, FF=D_FF, do_wout=True, do_norm=True,
                                                            do_ffn=True, ffn_mode="res", do_next=has_next))
            wo = np.asarray(w_out[layer], f32); g_ = np.asarray(ffn_w_gate[i], f32); u_ = np.asarray(ffn_w_up[i], f32); d_ = np.asarray(ffn_w_down[i], f32)
            maps = []
            for c in range(NCORE):
                m = dict(c_ones=ones, xT=xT[c], mixT=mixT[c], w_out=wo, n2w=_lay(norm2_w[layer]), wg=g_, wu=u_, wd=d_)
                if has_next:
                    m["n1w"] = _lay(norm1_w[layer + 1])
                maps.append(m)
            r = _run(nc, maps)
            xT = [r[c]["yT"] for c in range(NCORE)]
            if has_next:
                hT_all = np.concatenate([r[c]["hnT"] for c in range(NCORE)], axis=1)
        else:
            i = layer // 2
            nc = _nc("c1a", lambda nc: build_tok(nc, D, TC, DMIX=D, do_wout=True, do_norm=True, do_router=True))
            wo = np.asarray(w_out[layer], f32)
            r = _run(nc, [dict(c_ones=ones, xT=xT[c], mixT=mixT[c], w_out=wo, n2w=_lay(norm2_w[layer]),
                               rw=np.asarray(router_w[i], f32)) for c in range(NCORE)])
            x1T = [r[c]["x1T"] for c in range(NCORE)]
            h2T_all = np.concatenate([r[c]["hT_out"] for c in range(NCORE)], axis=1)
            comb_all = np.concatenate([r[c]["comb_out"] for c in range(NCORE)], axis=0)
            del r
            nc = _nc("moe", lambda nc: build_tok(nc, D, T, FF=MOE_FF, h_in=True, do_ffn=True, ffn_mode="comb"))
            r = _run(nc, [dict(c_ones=ones, hT=h2T_all, wg=np.asarray(moe_w_gate[i][e], f32), wu=np.asarray(moe_w_up[i][e], f32),
                               wd=np.asarray(moe_w_down[i][e], f32),
                               comb=np.ascontiguousarray(np.broadcast_to(comb_all[:, e][None, :], (128, T)))) for e in range(N_EXP)])
            parts = [r[e]["yT"] for e in range(N_EXP)]
            del r
            nc = _nc("red", lambda nc: build_reduce(nc, D, TC, N_EXP))
            r = _run(nc, [dict(x1T=x1T[c], parts=np.ascontiguousarray(np.stack([parts[e][:, c * TC:(c + 1) * TC] for e in range(N_EXP)])))
                          for c in range(NCORE)])
            xT = [r[c]["yT"] for c in range(NCORE)]
            if layer + 1 < DEPTH:
                raise NotImplementedError
    out = np.concatenate([np.ascontiguousarray(xT[c].T) for c in range(NCORE)], axis=0).reshape(BATCH, SEQ, D)
    return out.astype(f32)
```

```python
import numpy as np
from contextlib import ExitStack
import concourse.bass as bass
import concourse.mybir as mybir

F32 = mybir.dt.float32
BF16 = mybir.dt.bfloat16
I32 = mybir.dt.int32
AF = mybir.ActivationFunctionType
ALU = mybir.AluOpType
AX = mybir.AxisListType

ENGS = ("pe", "act", "dve", "pool", "sp")


class Buf:
    def __init__(self, name):
        self.name = name
        self.w = None
        self.r = []
        self.dsem = None


class Rec:
    def __getattr__(self, name):
        def f(*a, **k):
            self.call = (name, a, k)
        return f


def _rec(fn):
    r = Rec(); fn(r)
    return r.call


class Prog:
    def __init__(self, nc):
        self.nc = nc
        self.es = ExitStack()
        self.pes = None
        self.ops = {e: [] for e in ENGS}
        self.cnt = {e: 0 for e in ENGS}
        self.sem = {e: self.es.enter_context(nc.semaphore("s_" + e)) for e in ENGS}
        self.dcnt = {}
        self.known = {e: {} for e in ENGS}
        self.bufs = []
        self.nd = 0

    def sb(self, name, shape, dt):
        es = self.pes if self.pes is not None else self.es
        self.nd += 1; name = "%s_u%d" % (name, self.nd)
        t = es.enter_context(self.nc.sbuf_tensor(name, list(shape), dt))
        b = Buf(name); b.t = t; self.bufs.append(b)
        return b

    def ps(self, name, shape, dt=F32):
        t = self.es.enter_context(self.nc.psum_tensor(name, list(shape), dt))
        b = Buf(name); b.t = t; self.bufs.append(b)
        return b

    def dram(self, name, shape, dt, kind="Internal", **kw):
        t = self.nc.dram_tensor(name, list(shape), dt, kind=kind, **kw)
        b = Buf(name); b.t = t; self.bufs.append(b)
        return b

    def _dsem(self, b):
        if b.dsem is None:
            b.dsem = self.es.enter_context(self.nc.semaphore("d%d" % self.nd)); self.nd += 1
            self.dcnt[b.dsem] = 0
        return b.dsem

    def _deps(self, eng, reads, writes):
        deps = []
        for b in reads:
            if b.w is not None: deps.append(b.w)
        for b in writes:
            if b.w is not None: deps.append(b.w)
            deps.extend(b.r)
        waits = {}
        for (sem, val, dma) in deps:
            if dma:
                val = max(val, self.dcnt[sem])
            if sem is self.sem["pe"] and eng == "pe":
                continue
            if self.known[eng].get(sem, 0) >= val:
                continue
            waits[sem] = max(waits.get(sem, 0), val)
        for sem, val in waits.items():
            self.known[eng][sem] = val
        return list(waits.items())

    def op(self, eng, fn, reads=(), writes=()):
        waits = self._deps(eng, reads, writes)
        self.cnt[eng] += 1
        tok = (self.sem[eng], self.cnt[eng], False)
        self.ops[eng].append((waits, _rec(fn), (self.sem[eng], 1)))
        for b in reads: b.r.append(tok)
        for b in writes: b.w = tok; b.r = []
        return tok

    def dma(self, q, fn, src, dst):
        waits = self._deps(q, [src], [dst])
        sem = self._dsem(dst)
        self.dcnt[sem] += 16
        tok = (sem, self.dcnt[sem], True)
        self.ops[q].append((waits, _rec(fn), (sem, 16)))
        src.r.append(tok)
        dst.w = tok; dst.r = []
        return tok

    def wait_all(self, eng, bufs):
        deps_w = self._deps(eng, bufs, bufs)
        if deps_w:
            self.ops[eng].append((deps_w, None, None))

    def barrier(self):
        for e in ENGS:
            waits = {}
            for e2 in ENGS:
                if self.cnt[e2] and self.known[e].get(self.sem[e2], 0) < self.cnt[e2] and e2 != e:
                    waits[self.sem[e2]] = self.cnt[e2]
            for sem, c in self.dcnt.items():
                if c and self.known[e].get(sem, 0) < c:
                    waits[sem] = c
            for sem, val in waits.items():
                self.known[e][sem] = val
            if waits:
                self.ops[e].append((list(waits.items()), None, None))

    def emit(self):
        nc = self.nc
        ops = self.ops
        with nc.Block() as block:
            def run(eng, lst):
                for waits, fn, inc in lst:
                    for sem, val in waits:
                        eng.wait_ge(sem, val)
                    if fn is not None:
                        ins = getattr(eng, fn[0])(*fn[1], **fn[2])
                        ins.then_inc(inc[0], inc[1])

            @block.tensor
            def _(e): run(e, ops["pe"])

            @block.scalar
            def _(e): run(e, ops["act"])

            @block.vector
            def _(e): run(e, ops["dve"])

            @block.gpsimd
            def _(e): run(e, ops["pool"])

            @block.sync
            def _(e): run(e, ops["sp"])
        self.ops = {e: [] for e in ENGS}

    def phase(self):
        prog = self
        class _Ph:
            def __enter__(s2):
                prog.pes = ExitStack()
            def __exit__(s2, *a):
                if a[0] is None:
                    prog.barrier()
                    prog.emit()
                prog.pes.close(); prog.pes = None
        return _Ph()

    def phase_begin(self):
        self.pes = ExitStack()

    def phase_end(self):
        self.barrier()
        self.emit()
        self.pes.close(); self.pes = None

    def finish(self):
        self.emit()
        self.es.close()


import numpy as np

HD = 128
VD = 256
NH = 2
NG = 2
HPG = 4
PD = 64
NS = 128
CK = 4
EPS = 1e-6


def consts_np():
    k = np.arange(128)
    mincl = (k[:, None] <= k[None, :]).astype(np.float32)
    ugt = (k[:, None] > k[None, :]).astype(np.float32)
    return {
        "c_ident": np.eye(128, dtype=np.float32),
        "c_ones": np.ones((128, 128), np.float32),
        "c_mincl": mincl,
        "c_ugt": ugt,
    }


def build_mixer(nc, D, S, lam_init):
    import ml_dtypes
    p = Prog(nc)
    DC = D // 128
    TT = 512 if S >= 512 else S
    NT = S // TT
    NB = S // 128
    din = lambda name, shape, dt=F32: p.dram(name, shape, dt, kind="ExternalInput")
    hT = din("hT", [D, S], BF16)
    wqk = din("wqk", [D, 2 * NH * 2 * HD])
    wxbc = din("wxbc", [D, NG * HPG * PD + 2 * NG * NS])
    wtok = din("wtok", [D, NH * VD + NG * HPG * PD + NG * HPG])
    NXB = NG * HPG * PD + 2 * NG * NS
    NXC = NXB // 128
    cw = din("conv_w", [128, NXC, CK])
    cb = din("conv_b", [128, NXC])
    qkw = din("qkw", [128, 2])
    lamv = din("lamv", [128, 4 * HD])
    sublnw = din("sublnw", [128, VD])
    dtb = din("dtb", [128, NG * HPG])
    alog = din("alog", [128, NG * HPG])
    dsk = din("dsk", [128, NG * HPG])
    snw = din("snw", [128, NG * HPG * PD])
    cid = din("c_ident", [128, 128]); cones = din("c_ones", [128, 128])
    cmin = din("c_mincl", [128, 128]); cug = din("c_ugt", [128, 128])
    mix = p.dram("mix", [S, NH * VD + NG * HPG * PD], BF16, kind="ExternalOutput")
    QT = p.dram("QT", [2 * NH, 128, S], BF16); KT = p.dram("KT", [2 * NH, 128, S], BF16)
    XBC = p.dram("XBC", [NXC, 128, S], BF16)
    VE = p.dram("VE", [S, NH, VD + 1], BF16)
    SZ = p.dram("SZ", [S, NG * HPG * PD], BF16)
    DT = p.dram("DTs", [S, NG * HPG], F32)

    def ld(name, src, shape, dt=F32, q="sp"):
        b = p.sb(name, shape, dt)
        p.dma(q, lambda e: e.dma_start(out=b.t[:], in_=src.t[:]), src, b)
        return b
    ones_f = ld("ones_f", cones, [128, 128]); mincl_f = ld("mincl_f", cmin, [128, 128]); ugt_f = ld("ugt_f", cug, [128, 128])
    ident_f = ld("ident_f", cid, [128, 128])
    ident_b = p.sb("ident_b", [128, 128], BF16); mincl_b = p.sb("mincl_b", [128, 128], BF16); ones_b = p.sb("ones_b", [128, 128], BF16)
    p.op("dve", lambda e: e.tensor_copy(out=ident_b.t[:], in_=ident_f.t[:]), [ident_f], [ident_b])
    p.op("dve", lambda e: e.tensor_copy(out=mincl_b.t[:], in_=mincl_f.t[:]), [mincl_f], [mincl_b])
    p.op("dve", lambda e: e.tensor_copy(out=ones_b.t[:], in_=ones_f.t[:]), [ones_f], [ones_b])
    cw_s = ld("cw_s", cw, [128, NXC, CK]); cb_s = ld("cb_s", cb, [128, NXC]); qkw_s = ld("qkw_s", qkw, [128, 2])
    lam_s = ld("lam_s", lamv, [128, 4 * HD]); sub_s = ld("sub_s", sublnw, [128, VD])
    dtb_s = ld("dtb_s", dtb, [128, NG * HPG]); alog_s = ld("alog_s", alog, [128, NG * HPG]); dsk_s = ld("dsk_s", dsk, [128, NG * HPG])
    snw_s = ld("snw_s", snw, [128, NG * HPG * PD])
    ltmp = p.sb("ltmp", [128, 2 * HD], F32); lsum = p.sb("lsum", [128, 2], F32); neglam = p.sb("neglam", [128, 1], F32)
    p.op("dve", lambda e: e.tensor_tensor(out=ltmp.t[:, 0:HD], in0=lam_s.t[:, 0:HD], in1=lam_s.t[:, HD:2 * HD], op=ALU.mult), [lam_s], [ltmp])
    p.op("dve", lambda e: e.tensor_tensor(out=ltmp.t[:, HD:2 * HD], in0=lam_s.t[:, 2 * HD:3 * HD], in1=lam_s.t[:, 3 * HD:4 * HD], op=ALU.mult), [lam_s], [ltmp])
    p.op("dve", lambda e: e.tensor_reduce(out=lsum.t[:, 0:1], in_=ltmp.t[:, 0:HD], axis=AX.X, op=ALU.add), [ltmp], [lsum])
    p.op("dve", lambda e: e.tensor_reduce(out=lsum.t[:, 1:2], in_=ltmp.t[:, HD:2 * HD], axis=AX.X, op=ALU.add), [ltmp], [lsum])
    p.op("act", lambda e: e.activation(out=lsum.t[:], in_=lsum.t[:], func=AF.Exp), [lsum], [lsum])
    p.op("dve", lambda e: e.tensor_tensor(out=neglam.t[:], in0=lsum.t[:, 1:2], in1=lsum.t[:, 0:1], op=ALU.subtract), [lsum], [neglam])
    p.op("dve", lambda e: e.tensor_scalar(out=neglam.t[:], in0=neglam.t[:], scalar1=-float(lam_init), scalar2=None, op0=ALU.add), [neglam], [neglam])
    p.op("dve", lambda e: e.tensor_scalar(out=sub_s.t[:], in0=sub_s.t[:], scalar1=float(1.0 - lam_init), scalar2=None, op0=ALU.mult), [sub_s], [sub_s])
    p.op("dve", lambda e: e.tensor_scalar(out=qkw_s.t[:, 0:1], in0=qkw_s.t[:, 0:1], scalar1=float(HD ** -0.5), scalar2=None, op0=ALU.mult), [qkw_s], [qkw_s])
    aneg = p.sb("aneg", [128, NG * HPG], F32)
    p.op("act", lambda e: e.activation(out=aneg.t[:], in_=alog_s.t[:], func=AF.Exp), [alog_s], [aneg])
    p.op("dve", lambda e: e.tensor_scalar(out=aneg.t[:], in0=aneg.t[:], scalar1=-1.0, scalar2=None, op0=ALU.mult), [aneg], [aneg])

    PA = [p.ps("pa%d" % i, [128, 512]) for i in range(2)]
    PO = [p.ps("po%d" % i, [128, 512]) for i in range(4)]
    PT = p.ps("pt", [128, 512], BF16)
    PS = p.ps("psm", [128, 512])


    def load_h(t):
        b = hbuf[t % 2]
        t = t % NT
        nsp = 4 if DC >= 4 else 1
        cs = DC // nsp
        for i in range(nsp):
            p.dma("sp" if i % 2 == 0 else "act", lambda e, i=i: e.dma_start(out=b.t[:, i * cs:(i + 1) * cs, :], in_=hT.t[i * cs * 128:(i + 1) * cs * 128, t * TT:(t + 1) * TT].rearrange("(c p) n -> p c n", p=128)), hT, b)
        return b

    p.phase_begin()
    hbuf = [p.sb("hbuf%d" % i, [128, DC, TT], BF16) for i in range(2)]
    with_w = p.sb("wbig", [128, DC, 1024 + 16], BF16)
    nqk = 2 * NH * 2
    for c in range(DC):
        p.dma("pool", lambda e, c=c: e.dma_start(out=with_w.t[:, c, 0:nqk * 128], in_=wqk.t[c * 128:(c + 1) * 128, :]), wqk, with_w)
    sq = [p.sb("sq%d" % i, [128, TT], F32) for i in range(2)]
    rs = [p.sb("rs%d" % i, [128, TT], F32) for i in range(2)]
    ob = [p.sb("ob%d" % i, [128, TT], BF16) for i in range(2)]
    it = 0
    for t in range(NT):
        hb = load_h(t)
        for j in range(nqk):
            pa = PA[it % 2]; s_ = sq[it % 2]; r_ = rs[it % 2]; o_ = ob[it % 2]; it += 1
            for c in range(DC):
                p.op("pe", lambda e, pa=pa, c=c, j=j, hb=hb: e.matmul(pa.t[:, 0:TT], with_w.t[:, c, j * 128:(j + 1) * 128], hb.t[:, c, :], start=(c == 0), stop=(c == DC - 1)), [with_w, hb], [pa])
            p.op("act", lambda e, pa=pa, s_=s_: e.activation(out=s_.t[:], in_=pa.t[:, 0:TT], func=AF.Square), [pa], [s_])
            p.op("pe", lambda e, s_=s_: e.matmul(PS.t[:, 0:TT], ones_f.t[:], s_.t[:], start=True, stop=True), [ones_f, s_], [PS])
            p.op("act", lambda e, r_=r_: e.activation(out=r_.t[:], in_=PS.t[:, 0:TT], func=AF.Sqrt, scale=1.0 / HD, bias=EPS), [PS], [r_])
            p.op("dve", lambda e, r_=r_: e.reciprocal(out=r_.t[:], in_=r_.t[:]), [r_], [r_])
            wcol = 0 if j < 2 * NH else 1
            p.op("dve", lambda e, pa=pa, r_=r_, o_=o_, wcol=wcol: e.scalar_tensor_tensor(out=o_.t[:], in0=pa.t[:, 0:TT], scalar=qkw_s.t[:, wcol:wcol + 1], in1=r_.t[:], op0=ALU.mult, op1=ALU.mult), [pa, r_, qkw_s], [o_])
            dst = QT if j < 2 * NH else KT
            jj = j % (2 * NH)
            p.dma("sp", lambda e, o_=o_, dst=dst, jj=jj, t=t: e.dma_start(out=dst.t[jj, :, t * TT:(t + 1) * TT], in_=o_.t[:]), o_, dst)

    p.phase_end()
    p.phase_begin()
    hbuf = [p.sb("hbuf%d" % i, [128, DC, TT], BF16) for i in range(2)]
    with_w = p.sb("wbig", [128, DC, 1024 + 16], BF16)
    ob = [p.sb("ob%d" % i, [128, TT], BF16) for i in range(2)]
    for c in range(DC):
        p.dma("pool", lambda e, c=c: e.dma_start(out=with_w.t[:, c, 0:NXB], in_=wxbc.t[c * 128:(c + 1) * 128, :]), wxbc, with_w)
    cbuf = [p.sb("cbuf%d" % j, [128, CK - 1 + TT], F32) for j in range(NXC)]
    for j in range(NXC):
        p.op("dve", lambda e, j=j: e.memset(cbuf[j].t[:], 0.0), [], [cbuf[j]])
    acc = [p.sb("acc%d" % i, [128, TT], F32) for i in range(2)]
    for t in range(NT):
        hb = load_h(NT + t)
        for j in range(NXC):
            pa = PA[it % 2]; a_ = acc[it % 2]; o_ = ob[it % 2]; it += 1
            for c in range(DC):
                p.op("pe", lambda e, pa=pa, c=c, j=j, hb=hb: e.matmul(pa.t[:, 0:TT], with_w.t[:, c, j * 128:(j + 1) * 128], hb.t[:, c, :], start=(c == 0), stop=(c == DC - 1)), [with_w, hb], [pa])
            cbj = cbuf[j]
            p.op("act", lambda e, pa=pa, cbj=cbj: e.copy(out=cbj.t[:, CK - 1:], in_=pa.t[:, 0:TT]), [pa], [cbj])
            p.op("dve", lambda e, a_=a_, cbj=cbj, j=j: e.tensor_scalar(out=a_.t[:], in0=cbj.t[:, 0:TT], scalar1=cw_s.t[:, j, 0:1], scalar2=cb_s.t[:, j:j + 1], op0=ALU.mult, op1=ALU.add), [cbj, cw_s, cb_s], [a_])
            for tap in range(1, CK):
                p.op("dve", lambda e, a_=a_, cbj=cbj, j=j, tap=tap: e.scalar_tensor_tensor(out=a_.t[:], in0=cbj.t[:, tap:tap + TT], scalar=cw_s.t[:, j, tap:tap + 1], in1=a_.t[:], op0=ALU.mult, op1=ALU.add), [cbj, cw_s, a_], [a_])
            p.op("act", lambda e, a_=a_, o_=o_: e.activation(out=o_.t[:], in_=a_.t[:], func=AF.Silu), [a_], [o_])
            p.dma("sp", lambda e, o_=o_, j=j, t=t: e.dma_start(out=XBC.t[j, :, t * TT:(t + 1) * TT], in_=o_.t[:]), o_, XBC)
            p.op("dve", lambda e, cbj=cbj: e.tensor_copy(out=cbj.t[:, 0:CK - 1], in_=cbj.t[:, TT:TT + CK - 1]), [cbj], [cbj])

    p.phase_end()
    p.phase_begin()
    hbuf = [p.sb("hbuf%d" % i, [128, DC, TT], BF16) for i in range(2)]
    with_w = p.sb("wbig", [128, DC, 1024 + 16], BF16)
    NTK = NH * VD + NG * HPG * PD + NG * HPG
    for c in range(DC):
        p.dma("pool", lambda e, c=c: e.dma_start(out=with_w.t[:, c, 0:NTK], in_=wtok.t[c * 128:(c + 1) * 128, :]), wtok, with_w)
    vo = [p.sb("vo%d" % i, [128, NH, VD + 1], BF16) for i in range(2)]
    zo = [p.sb("zo%d" % i, [128, NG * HPG * PD], BF16) for i in range(2)]
    do = [p.sb("do%d" % i, [128, NG * HPG], F32) for i in range(2)]
    for i in range(2):
        p.op("dve", lambda e, i=i: e.memset(vo[i].t[:], 1.0), [], [vo[i]])
    zoff = NH * VD; doff = zoff + NG * HPG * PD
    for t in range(NT):
        hb = load_h(2 * NT + t)
        for s in range(TT // 128):
            blk = t * (TT // 128) + s
            pv = PA[0]; pz = PA[1]; pd = PS
            v_ = vo[blk % 2]; z_ = zo[blk % 2]; d_ = do[blk % 2]
            for (pp, lo, n) in ((pv, 0, NH * VD), (pz, zoff, NG * HPG * PD), (pd, doff, NG * HPG)):
                for c in range(DC):
                    p.op("pe", lambda e, pp=pp, c=c, lo=lo, n=n, hb=hb, s=s: e.matmul(pp.t[:, 0:n], hb.t[:, c, s * 128:(s + 1) * 128], with_w.t[:, c, lo:lo + n], start=(c == 0), stop=(c == DC - 1)), [with_w, hb], [pp])
            p.op("act", lambda e, v_=v_, pv=pv: e.copy(out=v_.t[:, :, 0:VD], in_=pv.t[:, 0:NH * VD].rearrange("p (h e) -> p h e", h=NH)), [pv], [v_])
            p.dma("sp", lambda e, v_=v_, blk=blk: e.dma_start(out=VE.t[blk * 128:(blk + 1) * 128, :, :], in_=v_.t[:]), v_, VE)
            p.op("act", lambda e, z_=z_, pz=pz: e.activation(out=z_.t[:], in_=pz.t[:, 0:NG * HPG * PD], func=AF.Silu), [pz], [z_])
            p.dma("sp", lambda e, z_=z_, blk=blk: e.dma_start(out=SZ.t[blk * 128:(blk + 1) * 128, :], in_=z_.t[:]), z_, SZ)
            p.op("dve", lambda e, d_=d_, pd=pd: e.tensor_tensor(out=d_.t[:], in0=pd.t[:, 0:NG * HPG], in1=dtb_s.t[:], op=ALU.add), [pd, dtb_s], [d_])
            p.op("act", lambda e, d_=d_: e.activation(out=d_.t[:], in_=d_.t[:], func=AF.Exp), [d_], [d_])
            p.op("act", lambda e, d_=d_: e.activation(out=d_.t[:], in_=d_.t[:], func=AF.Ln, bias=1.0), [d_], [d_])
            p.dma("sp", lambda e, d_=d_, blk=blk: e.dma_start(out=DT.t[blk * 128:(blk + 1) * 128, :], in_=d_.t[:]), d_, DT)

    p.phase_end()
    p.phase_begin()
    QTILE = 256 if S >= 256 else S
    NQS = QTILE // 128
    kts = p.sb("kts", [128, 2, S], BF16)
    ves = p.sb("ves", [128, NB, VD + 1], BF16)
    qts = [p.sb("qts%d" % i, [128, 2, QTILE], BF16) for i in range(2)]
    pts = [p.sb("pts%d" % i, [128, QTILE], BF16) for i in range(4)]
    o1 = [p.sb("o1_%d" % i, [128, VD], F32) for i in range(2)]
    rr = [p.sb("rr%d" % i, [128, 4], F32) for i in range(2)]
    osq = p.sb("osq", [128, VD], F32)
    oo = [p.sb("oo%d" % i, [128, VD], BF16) for i in range(2)]
    ipt = 0; iq = 0; io = 0; ipt_box = [0]
    for hd in range(NH):
        p.dma("sp", lambda e, hd=hd: e.dma_start(out=kts.t[:], in_=KT.t[2 * hd:2 * hd + 2, :, :].rearrange("c p n -> p c n")), KT, kts)
        for b0 in range(0, NB, 4):
            b1 = min(NB, b0 + 4)
            p.dma("act", lambda e, hd=hd, b0=b0, b1=b1: e.dma_start(out=ves.t[:, b0:b1, :], in_=VE.t[b0 * 128:b1 * 128, hd, :].rearrange("(b p) e -> p b e", p=128)), VE, ves)
        for qi in range(S // QTILE):
            qt = qts[iq % 2]; iq += 1
            p.dma("sp", lambda e, qt=qt, hd=hd, qi=qi: e.dma_start(out=qt.t[:], in_=QT.t[2 * hd:2 * hd + 2, :, qi * QTILE:(qi + 1) * QTILE].rearrange("c p n -> p c n")), QT, qt)
            nkb = NQS * (qi + 1)
            steps = [(comp, kb) for comp in range(2) for kb in range(nkb)]
            SC = [PA[0], PA[1], PS]
            LOOK = 2
            slot = {}
            def emit_qk(i):
                comp, kb = steps[i]
                pa = SC[ipt_box[0] % 3]; ipt_box[0] += 1
                slot[i] = pa
                p.op("pe", lambda e: e.matmul(pa.t[:, 0:QTILE], kts.t[:, comp, kb * 128:(kb + 1) * 128], qt.t[:, comp, :], start=True, stop=True), [kts, qt], [pa])
            for i in range(min(LOOK, len(steps))):
                emit_qk(i)
            for i, (comp, kb) in enumerate(steps):
                if i + LOOK < len(steps):
                    emit_qk(i + LOOK)
                pa = slot.pop(i); pt = pts[ipt % len(pts)]; ipt += 1
                p.op("act", lambda e: e.activation(out=pt.t[:], in_=pa.t[:, 0:QTILE], func=AF.Exp), [pa], [pt])
                dq = kb - NQS * qi
                if dq >= 0:
                    p.op("dve", lambda e: e.tensor_tensor(out=pt.t[:, dq * 128:(dq + 1) * 128], in0=pt.t[:, dq * 128:(dq + 1) * 128], in1=mincl_b.t[:], op=ALU.mult), [pt, mincl_b], [pt])
                for qs in range(NQS):
                    last = NQS * qi + qs
                    if kb > last:
                        continue
                    po = PO[comp * 2 + qs]
                    p.op("pe", lambda e: e.matmul(po.t[:, 0:VD + 1], pt.t[:, qs * 128:(qs + 1) * 128], ves.t[:, kb, :], start=(kb == 0), stop=(kb == last)), [pt, ves], [po])
            for qs in range(NQS):
                r_ = rr[io % 2]; o_ = o1[io % 2]; ob_ = oo[io % 2]; io += 1
                pa_, pb_ = PO[qs], PO[2 + qs]
                p.op("dve", lambda e, r_=r_, pa_=pa_: e.reciprocal(out=r_.t[:, 0:1], in_=pa_.t[:, VD:VD + 1]), [pa_], [r_])
                p.op("dve", lambda e, r_=r_, pb_=pb_: e.reciprocal(out=r_.t[:, 1:2], in_=pb_.t[:, VD:VD + 1]), [pb_], [r_])
                p.op("dve", lambda e, r_=r_: e.tensor_tensor(out=r_.t[:, 1:2], in0=r_.t[:, 1:2], in1=neglam.t[:], op=ALU.mult), [r_, neglam], [r_])
                p.op("dve", lambda e, r_=r_, o_=o_, pa_=pa_: e.tensor_scalar(out=o_.t[:], in0=pa_.t[:, 0:VD], scalar1=r_.t[:, 0:1], scalar2=None, op0=ALU.mult), [pa_, r_], [o_])
                p.op("dve", lambda e, r_=r_, o_=o_, pb_=pb_: e.scalar_tensor_tensor(out=o_.t[:], in0=pb_.t[:, 0:VD], scalar=r_.t[:, 1:2], in1=o_.t[:], op0=ALU.mult, op1=ALU.add), [pb_, r_, o_], [o_])
                p.op("act", lambda e, o_=o_: e.activation(out=osq.t[:], in_=o_.t[:], func=AF.Square), [o_], [osq])
                p.op("dve", lambda e, r_=r_: e.tensor_reduce(out=r_.t[:, 2:3], in_=osq.t[:], axis=AX.X, op=ALU.add), [osq], [r_])
                p.op("act", lambda e, r_=r_: e.activation(out=r_.t[:, 2:3], in_=r_.t[:, 2:3], func=AF.Sqrt, scale=1.0 / VD, bias=EPS), [r_], [r_])
                p.op("dve", lambda e, r_=r_: e.reciprocal(out=r_.t[:, 2:3], in_=r_.t[:, 2:3]), [r_], [r_])
                p.op("dve", lambda e, r_=r_, o_=o_, ob_=ob_: e.scalar_tensor_tensor(out=ob_.t[:], in0=o_.t[:], scalar=r_.t[:, 2:3], in1=sub_s.t[:], op0=ALU.mult, op1=ALU.mult), [o_, r_, sub_s], [ob_])
                tok0 = qi * QTILE + qs * 128
                p.dma("sp", lambda e, ob_=ob_, tok0=tok0, hd=hd: e.dma_start(out=mix.t[tok0:tok0 + 128, hd * VD:(hd + 1) * VD], in_=ob_.t[:]), ob_, mix)

    p.phase_end()
    p.phase_begin()
    GW = HPG * PD
    for g in range(NG):
        stf = p.sb("stf%d" % g, [128, GW], F32); stb = p.sb("stb%d" % g, [128, GW], BF16)
        p.op("dve", lambda e, stf=stf: e.memset(stf.t[:], 0.0), [], [stf])
        p.op("dve", lambda e, stb=stb: e.memset(stb.t[:], 0.0), [], [stb])
        xch = [2 * g, 2 * g + 1]; bch = NG * 2 + g; cch = NG * 2 + NG + g
        nbuf = 2
        xt_ = [p.sb("xt%d_%d" % (g, i), [128, 2, 128], BF16) for i in range(nbuf)]
        bt_ = [p.sb("bt%d_%d" % (g, i), [128, 128], BF16) for i in range(nbuf)]
        ct_ = [p.sb("ct%d_%d" % (g, i), [128, 128], BF16) for i in range(nbuf)]
        dt_ = [p.sb("dt%d_%d" % (g, i), [128, HPG], F32) for i in range(nbuf)]
        sz_ = [p.sb("sz%d_%d" % (g, i), [128, GW], BF16) for i in range(nbuf)]
        xm = [p.sb("xm%d_%d" % (g, i), [128, GW], BF16) for i in range(nbuf)]
        bm = [p.sb("bm%d_%d" % (g, i), [128, 128], BF16) for i in range(nbuf)]
        dta = [p.sb("dta%d_%d" % (g, i), [128, HPG], F32) for i in range(nbuf)]
        sm = [p.sb("sm%d_%d" % (g, i), [128, 4 * HPG], F32) for i in range(nbuf)]
        cbm = [p.sb("cbm%d_%d" % (g, i), [128, 128], F32) for i in range(nbuf)]
        Rb = [p.sb("R%d_%d" % (g, i), [128, 128], F32) for i in range(2)]
        dec = [p.sb("dec%d_%d" % (g, i), [128, 128], F32) for i in range(2)]
        wt = [p.sb("wt%d_%d" % (g, i), [128, 128], BF16) for i in range(2)]
        eab = [p.sb("eab%d_%d" % (g, i), [128, 128], F32) for i in range(2)]
        cpr = [p.sb("cpr%d_%d" % (g, i), [128, 128], BF16) for i in range(2)]
        yf = [p.sb("yf%d_%d" % (g, i), [128, GW], F32) for i in range(2)]
        ysq = p.sb("ysq%d" % g, [128, GW], F32)
        yr = [p.sb("yr%d_%d" % (g, i), [128, 2], F32) for i in range(2)]
        yo = [p.sb("yo%d_%d" % (g, i), [128, GW], BF16) for i in range(2)]
        xw = [p.sb("xw%d_%d" % (g, i), [128, GW], BF16) for i in range(2)]
        ih = 0
        for c in range(NB):
            i = c % nbuf
            X_, B_, C_, D_, Z_ = xt_[i], bt_[i], ct_[i], dt_[i], sz_[i]
            sl = slice(c * 128, (c + 1) * 128)
            p.dma("sp", lambda e, X_=X_, sl=sl: e.dma_start(out=X_.t[:], in_=XBC.t[xch[0]:xch[0] + 2, :, sl].rearrange("c p n -> p c n")), XBC, X_)
            p.dma("sp", lambda e, B_=B_, sl=sl: e.dma_start(out=B_.t[:], in_=XBC.t[bch, :, sl]), XBC, B_)
            p.dma("act", lambda e, C_=C_, sl=sl: e.dma_start(out=C_.t[:], in_=XBC.t[cch, :, sl]), XBC, C_)
            p.dma("act", lambda e, D_=D_, sl=sl: e.dma_start(out=D_.t[:], in_=DT.t[sl, g * HPG:(g + 1) * HPG]), DT, D_)
            p.dma("act", lambda e, Z_=Z_, sl=sl: e.dma_start(out=Z_.t[:], in_=SZ.t[sl, g * GW:(g + 1) * GW]), SZ, Z_)
            XM, BM = xm[i], bm[i]
            for k in range(2):
                p.op("pe", lambda e, X_=X_, k=k: e.transpose(PT.t[:, k * 128:(k + 1) * 128], X_.t[:, k, :], ident_b.t[:]), [X_, ident_b], [PT])
            p.op("pe", lambda e, B_=B_: e.transpose(PT.t[:, 256:384], B_.t[:], ident_b.t[:]), [B_, ident_b], [PT])
            p.op("act", lambda e, XM=XM: e.copy(out=XM.t[:], in_=PT.t[:, 0:256]), [PT], [XM])
            p.op("act", lambda e, BM=BM: e.copy(out=BM.t[:], in_=PT.t[:, 256:384]), [PT], [BM])
            DA = dta[i]; SM = sm[i]
            p.op("dve", lambda e, DA=DA, D_=D_: e.tensor_tensor(out=DA.t[:], in0=D_.t[:], in1=aneg.t[:, g * HPG:(g + 1) * HPG], op=ALU.mult), [D_, aneg], [DA])
            p.op("pe", lambda e, DA=DA: e.matmul(PS.t[:, 0:HPG], mincl_f.t[:], DA.t[:], start=True, stop=True), [mincl_f, DA], [PS])
            p.op("pe", lambda e, DA=DA: e.matmul(PS.t[:, 8:8 + HPG], ones_f.t[:], DA.t[:], start=True, stop=True), [ones_f, DA], [PS])
            p.op("act", lambda e, SM=SM: e.copy(out=SM.t[:, 0:HPG], in_=PS.t[:, 0:HPG]), [PS], [SM])
            p.op("act", lambda e, SM=SM: e.activation(out=SM.t[:, HPG:2 * HPG], in_=PS.t[:, 8:8 + HPG], func=AF.Exp), [PS], [SM])
            p.op("dve", lambda e, SM=SM: e.tensor_tensor(out=SM.t[:, 2 * HPG:3 * HPG], in0=PS.t[:, 8:8 + HPG], in1=SM.t[:, 0:HPG], op=ALU.subtract), [PS, SM], [SM])
            p.op("act", lambda e, SM=SM: e.activation(out=SM.t[:, 2 * HPG:3 * HPG], in_=SM.t[:, 2 * HPG:3 * HPG], func=AF.Exp), [SM], [SM])
            p.op("dve", lambda e, SM=SM, D_=D_: e.tensor_tensor(out=SM.t[:, 2 * HPG:3 * HPG], in0=SM.t[:, 2 * HPG:3 * HPG], in1=D_.t[:], op=ALU.mult), [SM, D_], [SM])
            CB = cbm[i]
            p.op("pe", lambda e, B_=B_, C_=C_: e.matmul(PA[0].t[:, 0:128], B_.t[:], C_.t[:], start=True, stop=True), [B_, C_], [PA[0]])
            p.op("dve", lambda e, CB=CB: e.tensor_tensor(out=CB.t[:], in0=PA[0].t[:, 0:128], in1=mincl_f.t[:], op=ALU.mult), [PA[0], mincl_f], [CB])
            PY = PO[c % 2]
            for h in range(HPG):
                R_ = Rb[ih % 2]; DE = dec[ih % 2]; WT = wt[ih % 2]; EA = eab[ih % 2]; CP = cpr[ih % 2]; ih += 1
                pseg = PA[1]; pea = PO[2]
                p.op("dve", lambda e, R_=R_, DA=DA, h=h: e.tensor_scalar(out=R_.t[:], in0=mincl_f.t[:], scalar1=DA.t[:, h:h + 1], scalar2=None, op0=ALU.mult), [mincl_f, DA], [R_])
                p.op("pe", lambda e, R_=R_, pseg=pseg: e.matmul(pseg.t[:, 0:128], ugt_f.t[:], R_.t[:], start=True, stop=True), [ugt_f, R_], [pseg])
                p.op("pe", lambda e, R_=R_, pea=pea: e.matmul(pea.t[:, 0:128], ones_f.t[:], R_.t[:], start=True, stop=True), [ones_f, R_], [pea])
                p.op("act", lambda e, DE=DE, pseg=pseg: e.activation(out=DE.t[:], in_=pseg.t[:, 0:128], func=AF.Exp), [pseg], [DE])
                p.op("act", lambda e, EA=EA, pea=pea: e.activation(out=EA.t[:], in_=pea.t[:, 0:128], func=AF.Exp), [pea], [EA])
                p.op("dve", lambda e, WT=WT, DE=DE, D_=D_, CB=CB, h=h: e.scalar_tensor_tensor(out=WT.t[:], in0=DE.t[:], scalar=D_.t[:, h:h + 1], in1=CB.t[:], op0=ALU.mult, op1=ALU.mult), [DE, D_, CB], [WT])
                p.op("dve", lambda e, CP=CP, EA=EA, C_=C_: e.tensor_tensor(out=CP.t[:], in0=EA.t[:], in1=C_.t[:], op=ALU.mult), [EA, C_], [CP])
                p.op("pe", lambda e, PY=PY, WT=WT, XM=XM, h=h: e.matmul(PY.t[:, h * PD:(h + 1) * PD], WT.t[:], XM.t[:, h * PD:(h + 1) * PD], start=True, stop=False), [WT, XM], [PY])
                p.op("pe", lambda e, PY=PY, CP=CP, h=h: e.matmul(PY.t[:, h * PD:(h + 1) * PD], CP.t[:], stb.t[:, h * PD:(h + 1) * PD], start=False, stop=True), [CP, stb, PY], [PY])
            YF = yf[c % 2]; YR = yr[c % 2]; YO = yo[c % 2]
            for h in range(HPG):
                hs = slice(h * PD, (h + 1) * PD)
                p.op("dve", lambda e, YF=YF, XM=XM, PY=PY, hs=hs, h=h: e.scalar_tensor_tensor(out=YF.t[:, hs], in0=XM.t[:, hs], scalar=dsk_s.t[:, g * HPG + h:g * HPG + h + 1], in1=PY.t[:, hs], op0=ALU.mult, op1=ALU.add), [XM, dsk_s, PY], [YF])
            p.op("dve", lambda e, YF=YF, Z_=Z_: e.tensor_tensor(out=YF.t[:], in0=YF.t[:], in1=Z_.t[:], op=ALU.mult), [YF, Z_], [YF])
            p.op("act", lambda e, YF=YF: e.activation(out=ysq.t[:], in_=YF.t[:], func=AF.Square), [YF], [ysq])
            p.op("dve", lambda e, YR=YR: e.tensor_reduce(out=YR.t[:, 0:1], in_=ysq.t[:], axis=AX.X, op=ALU.add), [ysq], [YR])
            p.op("act", lambda e, YR=YR: e.activation(out=YR.t[:, 0:1], in_=YR.t[:, 0:1], func=AF.Sqrt, scale=1.0 / GW, bias=EPS), [YR], [YR])
            p.op("dve", lambda e, YR=YR: e.reciprocal(out=YR.t[:, 0:1], in_=YR.t[:, 0:1]), [YR], [YR])
            p.op("dve", lambda e, YO=YO, YF=YF, YR=YR: e.scalar_tensor_tensor(out=YO.t[:], in0=YF.t[:], scalar=YR.t[:, 0:1], in1=snw_s.t[:, g * GW:(g + 1) * GW], op0=ALU.mult, op1=ALU.mult), [YF, YR, snw_s], [YO])
            p.dma("sp", lambda e, YO=YO, sl=sl: e.dma_start(out=mix.t[sl, NH * VD + g * GW:NH * VD + (g + 1) * GW], in_=YO.t[:]), YO, mix)
            XW = xw[c % 2]
            for h in range(HPG):
                hs = slice(h * PD, (h + 1) * PD)
                p.op("dve", lambda e, XW=XW, XM=XM, SM=SM, hs=hs, h=h: e.tensor_scalar(out=XW.t[:, hs], in0=XM.t[:, hs], scalar1=SM.t[:, 2 * HPG + h:2 * HPG + h + 1], scalar2=None, op0=ALU.mult), [XM, SM], [XW])
            pst = PO[3]
            p.op("pe", lambda e, BM=BM, XW=XW: e.matmul(pst.t[:, 0:GW], BM.t[:], XW.t[:], start=True, stop=True), [BM, XW], [pst])
            for h in range(HPG):
                hs = slice(h * PD, (h + 1) * PD)
                p.op("dve", lambda e, SM=SM, hs=hs, h=h: e.scalar_tensor_tensor(out=stf.t[:, hs], in0=stf.t[:, hs], scalar=SM.t[:, HPG + h:HPG + h + 1], in1=pst.t[:, hs], op0=ALU.mult, op1=ALU.add), [stf, SM, pst], [stf])
            p.op("act", lambda e: e.copy(out=stb.t[:], in_=stf.t[:]), [stf], [stb])

    p.phase_end()
    p.finish()
    return nc


import numpy as np

EPS = 1e-6
NE = 8


def build_tok(nc, D, T, DMIX=0, FF=0, do_wout=False, do_norm=False, do_router=False, do_ffn=False,
              ffn_mode="res", do_next=False, h_in=False):
    p = Prog(nc)
    DC = D // 128
    TT = 512 if T >= 512 else T
    NT = T // TT
    din = lambda name, shape, dt=F32: p.dram(name, shape, dt, kind="ExternalInput")
    dout = lambda name, shape, dt=F32: p.dram(name, shape, dt, kind="ExternalOutput")
    cones = din("c_ones", [128, 128])
    if do_wout or do_norm:
        xT = din("xT", [D, T])
    if do_wout:
        MC = DMIX // 128
        mixT = din("mixT", [DMIX, T], BF16); w_out = din("w_out", [DMIX, D])
        WoB = p.dram("WoB", [DC, 128, MC, 128], BF16)
    if do_norm:
        n2w = din("n2w", [128, DC])
    if h_in:
        hTin = din("hT", [D, T], BF16)
    if do_router:
        rw = din("rw", [D, NE]); comb_out = dout("comb_out", [T, NE])
    if do_ffn:
        NJ = FF // 128
        wg = din("wg", [D, FF]); wu = din("wu", [D, FF]); wd = din("wd", [FF, D])
        WgB = p.dram("WgB", [NJ, 128, DC, 128], BF16); WuB = p.dram("WuB", [NJ, 128, DC, 128], BF16)
        WdB = p.dram("WdB", [DC, 128, NJ, 128], BF16)
        yT = dout("yT", [D, T])
        if ffn_mode == "comb":
            combb = din("comb", [128, T])
    if do_next:
        n1w = din("n1w", [128, DC]); hnT = dout("hnT", [D, T], BF16)
    need_x1_out = (do_wout or do_norm) and not (do_ffn and ffn_mode == "res")
    if do_wout:
        x1T = dout("x1T", [D, T]) if need_x1_out else p.dram("x1T", [D, T], F32)
    if do_norm and not do_ffn:
        hT_out = dout("hT_out", [D, T], BF16)

    ones_f = p.sb("ones_f", [128, 128], F32)
    p.dma("sp", lambda e: e.dma_start(out=ones_f.t[:], in_=cones.t[:]), cones, ones_f)
    if do_norm:
        n2w_s = p.sb("n2w_s", [128, DC], F32)
        p.dma("sp", lambda e: e.dma_start(out=n2w_s.t[:], in_=n2w.t[:]), n2w, n2w_s)
    if do_next:
        n1w_s = p.sb("n1w_s", [128, DC], F32)
        p.dma("sp", lambda e: e.dma_start(out=n1w_s.t[:], in_=n1w.t[:]), n1w, n1w_s)
    if do_router:
        rw_s = p.sb("rw_s", [128, DC, NE], F32)
        p.dma("sp", lambda e: e.dma_start(out=rw_s.t[:], in_=rw.t[:, :].rearrange("(c p) n -> p c n", p=128)), rw, rw_s)

    PW = [p.ps("pw%d" % i, [128, 512]) for i in range(2)]
    PG = [p.ps("pg%d" % i, [128, 512]) for i in range(2)]
    PU = [p.ps("pu%d" % i, [128, 512]) for i in range(2)]
    PSS = p.ps("pss", [128, 512])
    PR = p.ps("pr", [128, 512])

    qi = [0]
    def castq():
        qi[0] += 1
        return "pool"
    if do_wout:
        for fo in range(DC):
            for c0 in range(0, MC, 8):
                c1 = min(MC, c0 + 8)
                p.dma(castq(), lambda e, fo=fo, c0=c0, c1=c1: e.dma_start(out=WoB.t[fo, :, c0:c1, :], in_=w_out.t[c0 * 128:c1 * 128, fo * 128:(fo + 1) * 128].rearrange("(c p) n -> p c n", p=128)), w_out, WoB)
    if do_ffn:
        for j in range(NJ):
            for c0 in range(0, DC, 8):
                c1 = min(DC, c0 + 8)
                for (src, dst) in ((wg, WgB), (wu, WuB)):
                    p.dma(castq(), lambda e, j=j, c0=c0, c1=c1, src=src, dst=dst: e.dma_start(out=dst.t[j, :, c0:c1, :], in_=src.t[c0 * 128:c1 * 128, j * 128:(j + 1) * 128].rearrange("(c p) n -> p c n", p=128)), src, dst)
        for fo in range(DC):
            for j0 in range(0, NJ, 8):
                j1 = min(NJ, j0 + 8)
                p.dma(castq(), lambda e, fo=fo, j0=j0, j1=j1: e.dma_start(out=WdB.t[fo, :, j0:j1, :], in_=wd.t[j0 * 128:j1 * 128, fo * 128:(fo + 1) * 128].rearrange("(c p) n -> p c n", p=128)), wd, WdB)

    ACH = max(DC, (DMIX // 128) if do_wout else 0)
    A = p.sb("A", [128, ACH, TT], BF16)
    if do_ffn:
        HJ = (NJ + 1) // 2 if NJ > 44 else NJ
        WSZ = max(DC, HJ, (DMIX // 128) if do_wout else 0) * 128
        actT = p.sb("actT", [128, NJ, TT], BF16)
    else:
        WSZ = max(DC, (DMIX // 128) if do_wout else 1) * 128
    NWB = 3
    wb = [p.sb("wb%d" % i, [128, WSZ], BF16) for i in range(NWB)]
    xc = [p.sb("xc%d" % i, [128, TT], F32) for i in range(3)]
    sq = [p.sb("sq%d" % i, [128, TT], F32) for i in range(2)]
    sg = [p.sb("sg%d" % i, [128, TT], F32) for i in range(2)]
    hb16 = [p.sb("hb16_%d" % i, [128, TT], BF16) for i in range(2)]
    rstd = p.sb("rstd", [128, TT], F32)
    if ffn_mode == "comb" and do_ffn:
        cmb = p.sb("cmb", [128, TT], F32)
    if do_router:
        hf = [p.sb("hf%d" % i, [128, TT], F32) for i in range(2)]
        lg = p.sb("lg", [128, (TT // 128) * NE], F32)
        rt = [p.sb("rt%d" % i, [128, NE], F32) for i in range(4)]
        rm = p.sb("rm", [128, 4], F32)
    cnt = {"w": 0, "x": 0, "s": 0, "g": 0, "h": 0, "q": 0}

    def nxt(k, lst):
        b = lst[cnt[k] % len(lst)]; cnt[k] += 1
        return b

    def dq():
        cnt["q"] += 1
        return "sp" if cnt["q"] % 2 else "act"

    def rms_from_pss(dst):
        p.op("act", lambda e: e.activation(out=dst.t[:], in_=PSS.t[:, 0:TT], func=AF.Sqrt, scale=1.0 / D, bias=EPS), [PSS], [dst])
        p.op("dve", lambda e: e.reciprocal(out=dst.t[:], in_=dst.t[:]), [dst], [dst])

    def accum_sumsq(src, first, last):
        s_ = nxt("s", sq)
        p.op("act", lambda e: e.activation(out=s_.t[:], in_=src.t[:], func=AF.Square), [src], [s_])
        p.op("pe", lambda e: e.matmul(PSS.t[:, 0:TT], ones_f.t[:], s_.t[:], start=first, stop=last), [ones_f, s_], [PSS])

    for t in range(NT):
        ts = slice(t * TT, (t + 1) * TT)
        if do_wout:
            for c0 in range(0, MC, 8):
                c1 = min(MC, c0 + 8)
                p.dma(dq(), lambda e, c0=c0, c1=c1: e.dma_start(out=A.t[:, c0:c1, :], in_=mixT.t[c0 * 128:c1 * 128, ts].rearrange("(c p) n -> p c n", p=128)), mixT, A)
            for fo in range(DC):
                w_ = nxt("w", wb); pw = PW[fo % 2]; x_ = nxt("x", xc)
                p.dma(dq(), lambda e: e.dma_start(out=w_.t[:, 0:MC * 128], in_=WoB.t[fo, :, :, :].rearrange("p c n -> p (c n)")), WoB, w_)
                p.dma(dq(), lambda e: e.dma_start(out=x_.t[:], in_=xT.t[fo * 128:(fo + 1) * 128, ts]), xT, x_)
                for c in range(MC):
                    p.op("pe", lambda e, c=c: e.matmul(pw.t[:, 0:TT], w_.t[:, c * 128:(c + 1) * 128], A.t[:, c, :], start=(c == 0), stop=(c == MC - 1)), [w_, A], [pw])
                p.op("dve", lambda e: e.tensor_tensor(out=x_.t[:], in0=pw.t[:, 0:TT], in1=x_.t[:], op=ALU.add), [pw, x_], [x_])
                p.dma(dq(), lambda e: e.dma_start(out=x1T.t[fo * 128:(fo + 1) * 128, ts], in_=x_.t[:]), x_, x1T)
                if do_norm:
                    accum_sumsq(x_, fo == 0, fo == DC - 1)
        elif do_norm:
            for fo in range(DC):
                x_ = nxt("x", xc)
                p.dma(dq(), lambda e: e.dma_start(out=x_.t[:], in_=xT.t[fo * 128:(fo + 1) * 128, ts]), xT, x_)
                accum_sumsq(x_, fo == 0, fo == DC - 1)
        if do_norm:
            rms_from_pss(rstd)
            srcT = x1T if do_wout else xT
            for c in range(DC):
                x_ = nxt("x", xc)
                p.dma(dq(), lambda e: e.dma_start(out=x_.t[:], in_=srcT.t[c * 128:(c + 1) * 128, ts]), srcT, x_)
                if do_router:
                    h_ = nxt("h", hf)
                    p.op("dve", lambda e: e.scalar_tensor_tensor(out=h_.t[:], in0=x_.t[:], scalar=n2w_s.t[:, c:c + 1], in1=rstd.t[:], op0=ALU.mult, op1=ALU.mult), [x_, n2w_s, rstd], [h_])
                    p.op("act", lambda e: e.copy(out=A.t[:, c, :], in_=h_.t[:]), [h_], [A])
                    for s in range(TT // 128):
                        prs = [PR, PG[0], PG[1], PU[0]][s]
                        p.op("pe", lambda e, s=s, prs=prs: e.matmul(prs.t[:, 0:NE], h_.t[:, s * 128:(s + 1) * 128], rw_s.t[:, c, :], start=(c == 0), stop=(c == DC - 1)), [h_, rw_s], [prs])
                else:
                    p.op("dve", lambda e: e.scalar_tensor_tensor(out=A.t[:, c, :], in0=x_.t[:], scalar=n2w_s.t[:, c:c + 1], in1=rstd.t[:], op0=ALU.mult, op1=ALU.mult), [x_, n2w_s, rstd], [A])
                if not do_ffn:
                    hb = nxt("g", hb16)
                    p.op("act", lambda e: e.copy(out=hb.t[:], in_=A.t[:, c, :]), [A], [hb])
                    p.dma(dq(), lambda e: e.dma_start(out=hT_out.t[c * 128:(c + 1) * 128, ts], in_=hb.t[:]), hb, hT_out)
            if do_router:
                for s in range(TT // 128):
                    prs = [PR, PG[0], PG[1], PU[0]][s]
                    p.op("act", lambda e, s=s, prs=prs: e.copy(out=lg.t[:, s * NE:(s + 1) * NE], in_=prs.t[:, 0:NE]), [prs], [lg])
                for s in range(TT // 128):
                    L = lg.t[:, s * NE:(s + 1) * NE]
                    e1, msk, e2, cb_ = rt
                    p.op("dve", lambda e: e.tensor_reduce(out=rm.t[:, 0:1], in_=L, axis=AX.X, op=ALU.max), [lg], [rm])
                    p.op("dve", lambda e: e.tensor_scalar(out=e1.t[:], in0=L, scalar1=rm.t[:, 0:1], scalar2=None, op0=ALU.is_equal), [lg, rm], [e1])
                    p.op("dve", lambda e: e.scalar_tensor_tensor(out=msk.t[:], in0=e1.t[:], scalar=-1e30, in1=L, op0=ALU.mult, op1=ALU.add), [e1, lg], [msk])
                    p.op("dve", lambda e: e.tensor_reduce(out=rm.t[:, 1:2], in_=msk.t[:], axis=AX.X, op=ALU.max), [msk], [rm])
                    p.op("dve", lambda e: e.tensor_scalar(out=e2.t[:], in0=msk.t[:], scalar1=rm.t[:, 1:2], scalar2=None, op0=ALU.is_equal), [msk, rm], [e2])
                    p.op("dve", lambda e: e.tensor_tensor(out=rm.t[:, 2:3], in0=rm.t[:, 0:1], in1=rm.t[:, 1:2], op=ALU.subtract), [rm], [rm])
                    p.op("act", lambda e: e.activation(out=rm.t[:, 3:4], in_=rm.t[:, 2:3], func=AF.Sigmoid, scale=-1.0), [rm], [rm])
                    p.op("act", lambda e: e.activation(out=rm.t[:, 2:3], in_=rm.t[:, 2:3], func=AF.Sigmoid), [rm], [rm])
                    p.op("dve", lambda e: e.tensor_scalar(out=cb_.t[:], in0=e1.t[:], scalar1=rm.t[:, 2:3], scalar2=None, op0=ALU.mult), [e1, rm], [cb_])
                    p.op("dve", lambda e: e.scalar_tensor_tensor(out=cb_.t[:], in0=e2.t[:], scalar=rm.t[:, 3:4], in1=cb_.t[:], op0=ALU.mult, op1=ALU.add), [e2, rm, cb_], [cb_])
                    p.dma(dq(), lambda e, s=s: e.dma_start(out=comb_out.t[t * TT + s * 128:t * TT + (s + 1) * 128, :], in_=cb_.t[:]), cb_, comb_out)
        if h_in:
            for c0 in range(0, DC, 8):
                c1 = min(DC, c0 + 8)
                p.dma(dq(), lambda e, c0=c0, c1=c1: e.dma_start(out=A.t[:, c0:c1, :], in_=hTin.t[c0 * 128:c1 * 128, ts].rearrange("(c p) n -> p c n", p=128)), hTin, A)
        if do_ffn:
            if ffn_mode == "comb":
                p.dma(dq(), lambda e: e.dma_start(out=cmb.t[:], in_=combb.t[:, ts]), combb, cmb)
            for j in range(NJ):
                wg_ = nxt("w", wb); wu_ = nxt("w", wb); pg = PG[j % 2]; pu = PU[j % 2]; s_ = nxt("g", sg)
                p.dma(dq(), lambda e: e.dma_start(out=wg_.t[:, 0:DC * 128], in_=WgB.t[j, :, :, :].rearrange("p c n -> p (c n)")), WgB, wg_)
                p.dma(dq(), lambda e: e.dma_start(out=wu_.t[:, 0:DC * 128], in_=WuB.t[j, :, :, :].rearrange("p c n -> p (c n)")), WuB, wu_)
                for c in range(DC):
                    p.op("pe", lambda e, c=c: e.matmul(pg.t[:, 0:TT], wg_.t[:, c * 128:(c + 1) * 128], A.t[:, c, :], start=(c == 0), stop=(c == DC - 1)), [wg_, A], [pg])
                for c in range(DC):
                    p.op("pe", lambda e, c=c: e.matmul(pu.t[:, 0:TT], wu_.t[:, c * 128:(c + 1) * 128], A.t[:, c, :], start=(c == 0), stop=(c == DC - 1)), [wu_, A], [pu])
                p.op("act", lambda e: e.activation(out=s_.t[:], in_=pg.t[:, 0:TT], func=AF.Silu), [pg], [s_])
                p.op("dve", lambda e: e.tensor_tensor(out=actT.t[:, j, :], in0=s_.t[:], in1=pu.t[:, 0:TT], op=ALU.mult), [s_, pu], [actT])
            for fo in range(DC):
                pw = PW[fo % 2]; x_ = nxt("x", xc)
                if ffn_mode == "res":
                    p.dma(dq(), lambda e: e.dma_start(out=x_.t[:], in_=x1T.t[fo * 128:(fo + 1) * 128, ts]), x1T, x_)
                for j0 in range(0, NJ, HJ):
                    j1 = min(NJ, j0 + HJ)
                    w_ = nxt("w", wb)
                    p.dma(dq(), lambda e: e.dma_start(out=w_.t[:, 0:(j1 - j0) * 128], in_=WdB.t[fo, :, j0:j1, :].rearrange("p c n -> p (c n)")), WdB, w_)
                    for j in range(j0, j1):
                        p.op("pe", lambda e, j=j: e.matmul(pw.t[:, 0:TT], w_.t[:, (j - j0) * 128:(j - j0 + 1) * 128], actT.t[:, j, :], start=(j == 0), stop=(j == NJ - 1)), [w_, actT], [pw])
                if ffn_mode == "res":
                    p.op("dve", lambda e: e.tensor_tensor(out=x_.t[:], in0=pw.t[:, 0:TT], in1=x_.t[:], op=ALU.add), [pw, x_], [x_])
                else:
                    p.op("dve", lambda e: e.tensor_tensor(out=x_.t[:], in0=pw.t[:, 0:TT], in1=cmb.t[:], op=ALU.mult), [pw, cmb], [x_])
                p.dma(dq(), lambda e: e.dma_start(out=yT.t[fo * 128:(fo + 1) * 128, ts], in_=x_.t[:]), x_, yT)
                if do_next:
                    accum_sumsq(x_, fo == 0, fo == DC - 1)
            if do_next:
                rms_from_pss(rstd)
                for c in range(DC):
                    x_ = nxt("x", xc); hb = nxt("h", hb16)
                    p.dma(dq(), lambda e: e.dma_start(out=x_.t[:], in_=yT.t[c * 128:(c + 1) * 128, ts]), yT, x_)
                    p.op("dve", lambda e: e.scalar_tensor_tensor(out=hb.t[:], in0=x_.t[:], scalar=n1w_s.t[:, c:c + 1], in1=rstd.t[:], op0=ALU.mult, op1=ALU.mult), [x_, n1w_s, rstd], [hb])
                    p.dma(dq(), lambda e: e.dma_start(out=hnT.t[c * 128:(c + 1) * 128, ts], in_=hb.t[:]), hb, hnT)
    p.barrier()
    p.finish()
    return nc


def build_reduce(nc, D, T, NP):
    p = Prog(nc)
    DC = D // 128
    x1T = p.dram("x1T", [D, T], F32, kind="ExternalInput")
    parts = p.dram("parts", [NP, D, T], F32, kind="ExternalInput")
    yT = p.dram("yT", [D, T], F32, kind="ExternalOutput")
    TT = 1024 if T >= 1024 else T
    acc = [p.sb("acc%d" % i, [128, TT], F32) for i in range(2)]
    pb = [p.sb("pb%d" % i, [128, TT], F32) for i in range(4)]
    k = 0; q = 0
    for c in range(DC):
        for t in range(T // TT):
            ts = slice(t * TT, (t + 1) * TT)
            a_ = acc[k % 2]; k += 1
            p.dma("sp", lambda e: e.dma_start(out=a_.t[:], in_=x1T.t[c * 128:(c + 1) * 128, ts]), x1T, a_)
            for i in range(NP):
                b_ = pb[q % 4]; q += 1
                p.dma("act" if i % 2 else "sp", lambda e, i=i: e.dma_start(out=b_.t[:], in_=parts.t[i, c * 128:(c + 1) * 128, ts]), parts, b_)
                p.op("dve" if i % 2 else "pool", lambda e: e.tensor_tensor(out=a_.t[:], in0=a_.t[:], in1=b_.t[:], op=ALU.add), [a_, b_], [a_])
            p.dma("sp", lambda e: e.dma_start(out=yT.t[c * 128:(c + 1) * 128, ts], in_=a_.t[:]), a_, yT)
    p.barrier()
    p.finish()
    return nc


import ml_dtypes as _mld
from concourse.bass_utils import run_bass_kernel_spmd

D_MODEL = 4096; SEQ = 8192; BATCH = 2; DEPTH = 2
D_FF = 11008; MOE_FF = 5632; N_EXP = 8
NCORE = 8
_cache = {}


def _lam_init(layer):
    import math
    return 0.8 - 0.6 * math.exp(-0.3 * layer)


def _nc(key, builder):
    if key not in _cache:
        nc = bass.Bass("TRN2", target_bir_lowering=False)
        builder(nc)
        _cache[key] = nc
    return _cache[key]


def _run(nc, in_maps):
    res = run_bass_kernel_spmd(nc, in_maps, core_ids=list(range(NCORE)))
    return res.results


def _lay(w):
    return np.ascontiguousarray(np.asarray(w, np.float32).reshape(-1, 128).T)


def _bc(v):
    v = np.asarray(v, np.float32)
    return np.ascontiguousarray(np.broadcast_to(v[None, :], (128, v.shape[0])))


def kernel(x, norm1_w, w_in, w_out, q_norm_w, k_norm_w, lambda_q1, lambda_k1, lambda_q2,
           lambda_k2, subln_w, conv_w, conv_b, dt_bias, a_log, d_skip, ssm_norm_w, norm2_w,
           ffn_w_gate, ffn_w_up, ffn_w_down, router_w, moe_w_gate, moe_w_up, moe_w_down):
    f32 = np.float32
    D = D_MODEL; T = BATCH * SEQ; TC = T // NCORE
    ones = np.ones((128, 128), f32)
    cst = consts_np()
    X = np.asarray(x, f32).reshape(T, D)
    xT = [np.ascontiguousarray(X[c * TC:(c + 1) * TC].T) for c in range(NCORE)]
    nc = _nc("n0", lambda nc: build_tok(nc, D, TC, do_norm=True))
    r = _run(nc, [dict(c_ones=ones, xT=xT[c], n2w=_lay(norm1_w[0])) for c in range(NCORE)])
    hT_all = np.concatenate([r[c]["hT_out"] for c in range(NCORE)], axis=1)
    AW = 2048; SW = 2048
    for layer in range(DEPTH):
        W = np.asarray(w_in[layer], f32)
        nc = _nc(("mix", layer), lambda nc: build_mixer(nc, D, SEQ, _lam_init(layer)))
        maps = []
        for c in range(NCORE):
            b, q = divmod(c, 4)
            o_q, o_k, o_v, o_z, o_x = 0, AW, 2 * AW, 3 * AW, 3 * AW + SW
            o_B = o_x + SW; o_C = o_B + 1024; o_dt = o_x + 4096
            wqk = np.concatenate([W[:, o_q + 512 * q:o_q + 512 * (q + 1)], W[:, o_k + 512 * q:o_k + 512 * (q + 1)]], 1)
            wxbc = np.concatenate([W[:, o_x + 512 * q:o_x + 512 * (q + 1)], W[:, o_B + 256 * q:o_B + 256 * (q + 1)],
                                   W[:, o_C + 256 * q:o_C + 256 * (q + 1)]], 1)
            wtok = np.concatenate([W[:, o_v + 512 * q:o_v + 512 * (q + 1)], W[:, o_z + 512 * q:o_z + 512 * (q + 1)],
                                   W[:, o_dt + 8 * q:o_dt + 8 * (q + 1)]], 1)
            cw = np.asarray(conv_w[layer], f32); cb = np.asarray(conv_b[layer], f32)
            csel = np.concatenate([np.arange(512 * q, 512 * (q + 1)), SW + np.arange(256 * q, 256 * (q + 1)),
                                   SW + 1024 + np.arange(256 * q, 256 * (q + 1))])
            cwq = cw[:, csel]; cbq = cb[csel]
            maps.append(dict(
                hT=np.ascontiguousarray(hT_all[:, b * SEQ:(b + 1) * SEQ]),
                wqk=np.ascontiguousarray(wqk), wxbc=np.ascontiguousarray(wxbc), wtok=np.ascontiguousarray(wtok),
                conv_w=np.ascontiguousarray(cwq.reshape(4, 8, 128).transpose(2, 1, 0)),
                conv_b=np.ascontiguousarray(cbq.reshape(8, 128).T),
                qkw=np.ascontiguousarray(np.stack([np.asarray(q_norm_w[layer], f32), np.asarray(k_norm_w[layer], f32)], 1)),
                lamv=_bc(np.concatenate([np.asarray(a[layer], f32) for a in (lambda_q1, lambda_k1, lambda_q2, lambda_k2)])),
                sublnw=_bc(subln_w[layer]), dtb=_bc(np.asarray(dt_bias[layer])[8 * q:8 * (q + 1)]),
                alog=_bc(np.asarray(a_log[layer])[8 * q:8 * (q + 1)]), dsk=_bc(np.asarray(d_skip[layer])[8 * q:8 * (q + 1)]),
                snw=_bc(np.asarray(ssm_norm_w[layer])[512 * q:512 * (q + 1)]), **cst))
        r = _run(nc, maps)
        mixfull = np.empty((T, D), _mld.bfloat16)
        for c in range(NCORE):
            b, q = divmod(c, 4)
            m = r[c]["mix"]
            mixfull[b * SEQ:(b + 1) * SEQ, 512 * q:512 * (q + 1)] = m[:, 0:512]
            mixfull[b * SEQ:(b + 1) * SEQ, AW + 512 * q:AW + 512 * (q + 1)] = m[:, 512:1024]
        mixT = [np.ascontiguousarray(mixfull[c * TC:(c + 1) * TC].T) for c in range(NCORE)]
        del mixfull, r
        if layer % 2 == 0:
            i = layer // 2
            has_next = layer + 1 < DEPTH
            nc = _nc(("c0", has_next), lambda nc: build_tok(nc, D, TC, DMIX=D, FF=D_FF, do_wout=True, do_norm=True,
                                                            do_ffn=True, ffn_mode="res", do_next=has_next))
            wo = np.asarray(w_out[layer], f32); g_ = np.asarray(ffn_w_gate[i], f32); u_ = np.asarray(ffn_w_up[i], f32); d_ = np.asarray(ffn_w_down[i], f32)
            maps = []
            for c in range(NCORE):
                m = dict(c_ones=ones, xT=xT[c], mixT=mixT[c], w_out=wo, n2w=_lay(norm2_w[layer]), wg=g_, wu=u_, wd=d_)
                if has_next:
                    m["n1w"] = _lay(norm1_w[layer + 1])
                maps.append(m)
            r = _run(nc, maps)
            xT = [r[c]["yT"] for c in range(NCORE)]
            if has_next:
                hT_all = np.concatenate([r[c]["hnT"] for c in range(NCORE)], axis=1)
        else:
            i = layer // 2
            nc = _nc("c1a", lambda nc: build_tok(nc, D, TC, DMIX=D, do_wout=True, do_norm=True, do_router=True))
            wo = np.asarray(w_out[layer], f32)
            r = _run(nc, [dict(c_ones=ones, xT=xT[c], mixT=mixT[c], w_out=wo, n2w=_lay(norm2_w[layer]),
                               rw=np.asarray(router_w[i], f32)) for c in range(NCORE)])
            x1T = [r[c]["x1T"] for c in range(NCORE)]
            h2T_all = np.concatenate([r[c]["hT_out"] for c in range(NCORE)], axis=1)
            comb_all = np.concatenate([r[c]["comb_out"] for c in range(NCORE)], axis=0)
            del r
            nc = _nc("moe", lambda nc: build_tok(nc, D, T, FF=MOE_FF, h_in=True, do_ffn=True, ffn_mode="comb"))
            r = _run(nc, [dict(c_ones=ones, hT=h2T_all, wg=np.asarray(moe_w_gate[i][e], f32), wu=np.asarray(moe_w_up[i][e], f32),
                               wd=np.asarray(moe_w_down[i][e], f32),
                               comb=np.ascontiguousarray(np.broadcast_to(comb_all[:, e][None, :], (128, T)))) for e in range(N_EXP)])
            parts = [r[e]["yT"] for e in range(N_EXP)]
            del r
            nc = _nc("red", lambda nc: build_reduce(nc, D, TC, N_EXP))
            r = _run(nc, [dict(x1T=x1T[c], parts=np.ascontiguousarray(np.stack([parts[e][:, c * TC:(c + 1) * TC] for e in range(N_EXP)])))
                          for c in range(NCORE)])
            xT = [r[c]["yT"] for c in range(NCORE)]
            if layer + 1 < DEPTH:
                raise NotImplementedError
    out = np.concatenate([np.ascontiguousarray(xT[c].T) for c in range(NCORE)], axis=0).reshape(BATCH, SEQ, D)
    return out.astype(f32)
```
